# Optimizing a Trainium2 kernel written in Bass

```python
import math
import jax, jax.numpy as jnp
from jax import lax
import numpy as np

D_MODEL = 1024
BATCH = 4
SEQ = 8192
DEPTH = 2
DEC_BATCH = 4
DEC_SEQ = 4096
PAST_LEN = 128

MIX_WIDTH = D_MODEL
GDN_HEAD_DIM = 128
GDN_WIDTH = MIX_WIDTH // 2
GDN_HEADS = GDN_WIDTH // GDN_HEAD_DIM
ATT_HEAD_DIM = 64
ATT_WIDTH = MIX_WIDTH - GDN_WIDTH
ATT_HEADS = ATT_WIDTH // ATT_HEAD_DIM
CONV_WIDTH = 5
CHUNK = 64
DILATED_PATTERNS = ((128, 1), (512, 4), (2048, 16))
N_EXPERTS = 32
TOP_K = 4
D_FF = D_MODEL
SWIGLU_LIMIT = 7.0
SWIGLU_ALPHA = 1.702
MOE_BLOCK = 256
DEEPNORM_ALPHA = (2 * DEPTH) ** 0.25
DEEPNORM_BETA = (8 * DEPTH) ** -0.25
LN_EPS = 1e-5
NORM_EPS = 1e-6
NEG_INF = -1e30
IN_COLS = 4 * GDN_WIDTH + 4 * GDN_HEADS + 3 * ATT_WIDTH

kernel_name = 'hybrid_gdn_dilated_moe_encoder'


def layer_norm(x, gain=None, bias=None):
    xf = x.astype(jnp.float32)
    mu = jnp.mean(xf, axis=-1, keepdims=True)
    var = jnp.mean(jnp.square(xf - mu), axis=-1, keepdims=True)
    y = (xf - mu) * lax.rsqrt(var + LN_EPS)
    if gain is not None:
        y = y * gain.astype(jnp.float32) + bias.astype(jnp.float32)
    return y.astype(x.dtype)


def rms_norm(x):
    return x * lax.rsqrt(jnp.mean(jnp.square(x), axis=-1, keepdims=True) + NORM_EPS)


def l2_normalize(x):
    return x * lax.rsqrt(jnp.sum(jnp.square(x), axis=-1, keepdims=True) + NORM_EPS)


def centred_short_conv(x, w):
    c = x.shape[-1]
    pad = (CONV_WIDTH - 1) // 2
    return lax.conv_general_dilated(x, w.astype(x.dtype)[:, None, :], window_strides=(1,),
                                    padding=[(pad, pad)], dimension_numbers=('NWC', 'WIO', 'NWC'),
                                    feature_group_count=c)


def chunk_gated_delta_rule(q, k, v, g, beta):
    b, h, t, dk = q.shape
    dv = v.shape[-1]
    n = t // CHUNK
    q, k, v = (a.reshape(b, h, n, CHUNK, -1) for a in (q, k, v))
    g = jnp.cumsum(g.reshape(b, h, n, CHUNK), axis=-1)
    beta = beta.reshape(b, h, n, CHUNK)[..., None]
    incl = jnp.tril(jnp.ones((CHUNK, CHUNK), bool))
    strict = jnp.tril(jnp.ones((CHUNK, CHUNK), bool), -1)
    decay = jnp.exp(jnp.where(incl, g[..., :, None] - g[..., None, :], -jnp.inf))
    kb = k * beta
    lower = jnp.where(strict, jnp.einsum('bhnik,bhnjk->bhnij', kb, k) * decay, 0.0)
    eye = jnp.eye(CHUNK, dtype=q.dtype)
    rhs = jnp.concatenate([v * beta, kb * jnp.exp(g)[..., None]], axis=-1)
    sol = lax.linalg.triangular_solve(lower + eye, rhs, left_side=True, lower=True, unit_diagonal=True)
    u, w = sol[..., :dv], sol[..., dv:]
    intra = jnp.where(incl, jnp.einsum('bhnik,bhnjk->bhnij', q, k) * decay, 0.0)
    q_dec = q * jnp.exp(g)[..., None]
    k_dec = k * jnp.exp(g[..., -1:] - g)[..., None]
    g_last = jnp.exp(g[..., -1])[..., None, None]

    def step(state, xs):
        q_i, k_i, u_i, w_i, a_i, gl_i = xs
        v_new = u_i - jnp.einsum('bhck,bhkv->bhcv', w_i, state)
        o_i = jnp.einsum('bhck,bhkv->bhcv', q_i, state) + jnp.einsum('bhcs,bhsv->bhcv', a_i, v_new)
        state = state * gl_i + jnp.einsum('bhck,bhcv->bhkv', k_i, v_new)
        return state, o_i

    xs = tuple(jnp.moveaxis(a, 2, 0) for a in (q_dec, k_dec, u, w, intra, g_last))
    _, o = lax.scan(step, jnp.zeros((b, h, dk, dv), q.dtype), xs)
    return jnp.moveaxis(o, 0, 2).reshape(b, h, t, dv)


def gated_deltanet(p, conv_w, a_log, dt_bias, gdn_norm_w):
    bsz, t, _ = p.shape
    f32 = jnp.float32
    qkv = jax.nn.silu(centred_short_conv(p[..., :3 * GDN_WIDTH], conv_w))
    z = p[..., 3 * GDN_WIDTH:4 * GDN_WIDTH].astype(f32).reshape(bsz, t, GDN_HEADS, GDN_HEAD_DIM)
    ab = p[..., 4 * GDN_WIDTH:].astype(f32).reshape(bsz, t, 2, 2, GDN_HEADS)
    heads = qkv.astype(f32).reshape(bsz, t, 3, GDN_HEADS, GDN_HEAD_DIM).transpose(2, 0, 3, 1, 4)
    q = l2_normalize(heads[0]) * (GDN_HEAD_DIM ** -0.5)
    k = l2_normalize(heads[1])
    v = heads[2]
    g = (-jnp.exp(a_log.astype(f32)) * jax.nn.softplus(ab[:, :, 0] + dt_bias.astype(f32))).transpose(2, 0, 3, 1)
    beta = jax.nn.sigmoid(ab[:, :, 1]).transpose(2, 0, 3, 1)
    flip = lambda a: jnp.flip(a, axis=2)
    o_fwd = chunk_gated_delta_rule(q, k, v, g[0], beta[0])
    o_bwd = flip(chunk_gated_delta_rule(flip(q), flip(k), flip(v), flip(g[1]), flip(beta[1])))
    o = (o_fwd + o_bwd).transpose(0, 2, 1, 3)
    return rms_norm(o) * gdn_norm_w.astype(f32) * jax.nn.silu(z)


def dilated_window_attention(q, k, v, half, dilation, slopes):
    bsz, t, h, dh = q.shape
    u = t // dilation
    to_res = lambda a: a.reshape(bsz, u, dilation, h, dh).transpose(0, 2, 3, 1, 4)
    qr, kr, vr = to_res(q), to_res(k), to_res(v)
    nb = -(-u // half)
    up = nb * half
    qb = jnp.pad(qr, ((0, 0), (0, 0), (0, 0), (0, up - u), (0, 0))).reshape(bsz, dilation, h, nb, half, dh)

    def band(a):
        ap = jnp.pad(a, ((0, 0), (0, 0), (0, 0), (half, up - u + half), (0, 0))).reshape(bsz, dilation, h, nb + 2, half, dh)
        return jnp.concatenate([ap[:, :, :, :-2], ap[:, :, :, 1:-1], ap[:, :, :, 2:]], axis=-2)

    kw, vw = band(kr), band(vr)
    s = jnp.einsum('brhnqd,brhnkd->brhnqk', qb, kw) * (dh ** -0.5)
    blk = jnp.arange(nb)[:, None, None] * half
    qpos = blk + jnp.arange(half)[None, :, None]
    kpos = blk + jnp.arange(3 * half)[None, None, :] - half
    rel = kpos - qpos
    valid = (jnp.abs(rel) <= half) & (kpos >= 0) & (kpos < u)
    dist = (dilation * jnp.abs(rel)).astype(jnp.float32)
    s = s - slopes[:, None, None, None] * dist
    s = jnp.where(valid, s, NEG_INF)
    mx = jnp.max(s, axis=-1, keepdims=True)
    lse = mx + jnp.log(jnp.sum(jnp.exp(s - mx), axis=-1, keepdims=True))
    o = jnp.einsum('brhnqk,brhnkd->brhnqd', jnp.exp(s - lse), vw)
    o = o.reshape(bsz, dilation, h, up, dh)[:, :, :, :u].transpose(0, 3, 1, 2, 4).reshape(bsz, t, h, dh)
    lse = lse[..., 0].reshape(bsz, dilation, h, up)[..., :u].transpose(0, 3, 1, 2).reshape(bsz, t, h)
    return o, lse


def dilated_attention(q, k, v):
    f32 = jnp.float32
    q, k, v = q.astype(f32), k.astype(f32), v.astype(f32)
    slopes = jnp.exp2(-8.0 * jnp.arange(1, ATT_HEADS + 1, dtype=f32) / ATT_HEADS)
    outs, lses = [], []
    for window, dilation in DILATED_PATTERNS:
        o, lse = dilated_window_attention(q, k, v, window // (2 * dilation), dilation, slopes)
        outs.append(o)
        lses.append(lse)
    weight = jax.nn.softmax(jnp.stack(lses, axis=0), axis=0)[..., None]
    return jnp.sum(weight * jnp.stack(outs, axis=0), axis=0)


def token_mixers(h, w_in, conv_w, a_log, dt_bias, gdn_norm_w, w_out):
    bsz, t, _ = h.shape
    proj = h @ w_in
    split = 4 * GDN_WIDTH + 4 * GDN_HEADS
    o_gdn = gated_deltanet(proj[..., :split], conv_w, a_log, dt_bias, gdn_norm_w)
    qkv = proj[..., split:].reshape(bsz, t, 3, ATT_HEADS, ATT_HEAD_DIM)
    o_att = dilated_attention(qkv[:, :, 0], qkv[:, :, 1], qkv[:, :, 2])
    o = jnp.concatenate([o_gdn.reshape(bsz, t, GDN_WIDTH), o_att.reshape(bsz, t, ATT_WIDTH)], axis=-1)
    return o.astype(h.dtype) @ w_out


def moe_ffn(h, router_w, router_b, w_gate_up, b_gate_up, w_down, b_down):
    bsz, t, d = h.shape
    xt = h.reshape(-1, d)
    n = xt.shape[0]
    logits = (xt @ router_w + router_b).astype(jnp.float32)
    top_logit, top_idx = lax.top_k(logits, TOP_K)
    top_w = jax.nn.softmax(top_logit, axis=-1)
    m = n * TOP_K
    flat_e = top_idx.reshape(-1)
    order = jnp.argsort(flat_e)
    sorted_e = flat_e[order]
    counts = jnp.bincount(flat_e, length=N_EXPERTS)
    padded = (counts + MOE_BLOCK - 1) // MOE_BLOCK * MOE_BLOCK
    pad_end = jnp.cumsum(padded)
    pad_start = pad_end - padded
    start = jnp.cumsum(counts) - counts
    dest = pad_start[sorted_e] + jnp.arange(m) - start[sorted_e]
    n_blocks = -(-m // MOE_BLOCK) + N_EXPERTS
    rows = n_blocks * MOE_BLOCK
    token = order // TOP_K
    row_token = jnp.full((rows,), n, jnp.int32).at[dest].set(token)
    x_pad = jnp.concatenate([xt, jnp.zeros((1, d), xt.dtype)], axis=0)
    xb = x_pad[row_token].reshape(n_blocks, MOE_BLOCK, d)
    block_e = jnp.minimum(jnp.searchsorted(pad_end, jnp.arange(n_blocks) * MOE_BLOCK, side='right'), N_EXPERTS - 1)

    def expert_block(args):
        xblk, e = args
        gu = xblk @ w_gate_up[e] + b_gate_up[e]
        gate = jnp.minimum(gu[:, :D_FF], SWIGLU_LIMIT)
        up = jnp.clip(gu[:, D_FF:], -SWIGLU_LIMIT, SWIGLU_LIMIT)
        act = (up + 1.0) * gate * jax.nn.sigmoid(SWIGLU_ALPHA * gate)
        return act @ w_down[e] + b_down[e]

    yb = lax.map(expert_block, (xb, block_e)).reshape(rows, d)
    contrib = yb[dest] * top_w.reshape(-1)[order][:, None].astype(yb.dtype)
    y = jax.ops.segment_sum(contrib, token, num_segments=n)
    return y.reshape(bsz, t, d)


def encoder_layer(x, c, w_ada, b_ada, w_in, conv_w, a_log, dt_bias, gdn_norm_w, w_out, ln1_g, ln1_b,
                  router_w, router_b, w_gate_up, b_gate_up, w_down, b_down, ln2_g, ln2_b):
    mod = (jax.nn.silu(c) @ w_ada + b_ada)[:, None, :]
    sh1, sc1, g1, sh2, sc2, g2 = jnp.split(mod, 6, axis=-1)
    h = layer_norm(x) * (1.0 + sc1) + sh1
    mix = token_mixers(h, w_in, conv_w, a_log, dt_bias, gdn_norm_w, w_out)
    x = layer_norm(DEEPNORM_ALPHA * x + g1 * mix, ln1_g, ln1_b)
    h = layer_norm(x) * (1.0 + sc2) + sh2
    ffn = moe_ffn(h, router_w, router_b, w_gate_up, b_gate_up, w_down, b_down)
    return layer_norm(DEEPNORM_ALPHA * x + g2 * ffn, ln2_g, ln2_b)


def setup_inputs(seed: int = 0) -> dict:
    key = jax.random.key(seed)
    ks = jax.random.split(key, 24)
    f32 = jnp.float32
    nrm = lambda k, shape, s: jax.random.normal(k, shape, f32) * s
    dt = jnp.exp(jax.random.uniform(ks[7], (DEPTH, 2, GDN_HEADS), f32, math.log(1e-3), math.log(1e-1)))
    return {
        'x_prompt': nrm(ks[0], (BATCH, SEQ, D_MODEL), 1.0),
        'x_sample': nrm(ks[1], (DEC_BATCH, DEC_SEQ, D_MODEL), 1.0),
        'c_prompt': nrm(ks[2], (BATCH, D_MODEL), 1.0),
        'c_sample': nrm(ks[3], (DEC_BATCH, D_MODEL), 1.0),
        'w_ada': nrm(ks[4], (DEPTH, D_MODEL, 6 * D_MODEL), D_MODEL ** -0.5),
        'b_ada': nrm(ks[5], (DEPTH, 6 * D_MODEL), 0.02),
        'w_in': nrm(ks[6], (DEPTH, D_MODEL, IN_COLS), D_MODEL ** -0.5),
        'conv_w': nrm(ks[8], (DEPTH, CONV_WIDTH, 3 * GDN_WIDTH), CONV_WIDTH ** -0.5),
        'a_log': jnp.log(jax.random.uniform(ks[9], (DEPTH, 2, GDN_HEADS), f32, 1.0, 16.0)),
        'dt_bias': jnp.log(jnp.expm1(dt)),
        'gdn_norm_w': 1.0 + nrm(ks[10], (DEPTH, GDN_HEAD_DIM), 0.02),
        'w_out': nrm(ks[11], (DEPTH, MIX_WIDTH, D_MODEL), MIX_WIDTH ** -0.5 * DEEPNORM_BETA),
        'ln1_g': 1.0 + nrm(ks[12], (DEPTH, D_MODEL), 0.02),
        'ln1_b': nrm(ks[13], (DEPTH, D_MODEL), 0.02),
        'router_w': nrm(ks[14], (DEPTH, D_MODEL, N_EXPERTS), D_MODEL ** -0.5),
        'router_b': nrm(ks[15], (DEPTH, N_EXPERTS), 0.01),
        'w_gate_up': nrm(ks[16], (DEPTH, N_EXPERTS, D_MODEL, 2 * D_FF), D_MODEL ** -0.5),
        'b_gate_up': nrm(ks[17], (DEPTH, N_EXPERTS, 2 * D_FF), 0.02),
        'w_down': nrm(ks[18], (DEPTH, N_EXPERTS, D_FF, D_MODEL), D_FF ** -0.5 * DEEPNORM_BETA),
        'b_down': nrm(ks[19], (DEPTH, N_EXPERTS, D_MODEL), 0.02),
        'ln2_g': 1.0 + nrm(ks[20], (DEPTH, D_MODEL), 0.02),
        'ln2_b': nrm(ks[21], (DEPTH, D_MODEL), 0.02),
    }


def reference(x_prompt, x_sample, c_prompt, c_sample, w_ada, b_ada, w_in, conv_w, a_log, dt_bias, gdn_norm_w,
              w_out, ln1_g, ln1_b, router_w, router_b, w_gate_up, b_gate_up, w_down, b_down, ln2_g, ln2_b):
    def trunk(x, c):
        for l in range(DEPTH):
            x = encoder_layer(x, c, w_ada[l], b_ada[l], w_in[l], conv_w[l], a_log[l], dt_bias[l], gdn_norm_w[l],
                              w_out[l], ln1_g[l], ln1_b[l], router_w[l], router_b[l], w_gate_up[l], b_gate_up[l],
                              w_down[l], b_down[l], ln2_g[l], ln2_b[l])
        return x

    y_prompt = trunk(x_prompt, c_prompt)
    y_sample = trunk(x_sample, c_sample)
    return (y_prompt, y_sample)
```

```python
import numpy as np
from contextlib import ExitStack
import ml_dtypes
import concourse.bass as bass
import concourse.mybir as mybir
from concourse.bass_utils import run_bass_kernel_spmd

F32 = mybir.dt.float32
BF16 = mybir.dt.bfloat16
AF = mybir.ActivationFunctionType
ALU = mybir.AluOpType
AX = mybir.AxisListType

D = 1024
NKT = 8
IN_COLS = 3600
NE = 32
DFF = 1024
ALPHA = 4.0 ** 0.25
LN_EPS = 1e-5
NORM_EPS = 1e-6
PAD = 1024
DILS = (1, 4, 16)
NEGBIG = -1.0e6
ENG = ('pe', 'act', 'dve', 'pool', 'sp')


def _bytes(dt):
    return 4 if dt == F32 else 2


class Buf:
    __slots__ = ('w', 'r', 'rd')

    def __init__(self):
        self.w = None
        self.r = {}
        self.rd = []


def bufs(n):
    return [Buf() for _ in range(n)]


class Sched:
    def __init__(self, nc, stack):
        self.nc = nc
        self.stack = stack
        self.streams = {e: [] for e in ENG}
        self.sem = {}
        self.cnt = {}
        self.nep = 0
        for e in ('pe', 'act', 'dve', 'pool'):
            self._epoch(e)
        self.seen = {e: {} for e in ENG}
        self.NDS = {'sp': 16, 'pool': 8, 'act': 4}
        self.dsem = {q: [stack.enter_context(nc.semaphore("dq%s%d" % (q, i))) for i in range(n)]
                     for q, n in self.NDS.items()}
        self.dk = {q: 0 for q in self.NDS}
        self.dtick = {q: [None] * n for q, n in self.NDS.items()}
        self.last = {}

    def _epoch(self, e):
        self.sem[e] = self.stack.enter_context(self.nc.semaphore("c%s%d" % (e, self.nep)))
        self.nep += 1
        self.cnt[e] = 0

    def op(self, eng, fn, R=(), W=(), dma=False):
        deps = []
        for b in R:
            if b.w is not None:
                deps.append(b.w)
        for b in W:
            if b.w is not None:
                deps.append(b.w)
            deps.extend(b.r.values())
            deps.extend(b.rd)
        if dma:
            nds = self.NDS[eng]
            dk = self.dk[eng]
            slot = dk % nds
            if self.dtick[eng][slot] is not None:
                deps.append(self.dtick[eng][slot])
            tick = (self.dsem[eng][slot], 16 * (dk // nds + 1), 'dma')
            self.dtick[eng][slot] = tick
            self.dk[eng] = dk + 1
        else:
            if self.cnt[eng] >= 30000:
                self._epoch(eng)
            self.cnt[eng] += 1
            tick = (self.sem[eng], self.cnt[eng], eng)
            self.last[eng] = tick
        waits = []
        seen = self.seen[eng]
        for (s, v, se) in deps:
            if se == 'pe' and eng == 'pe' and not dma:
                continue
            k = id(s)
            if seen.get(k, 0) >= v:
                continue
            seen[k] = v
            waits.append((s, v))
        self.streams[eng].append((waits, fn, tick))
        for b in R:
            if dma:
                b.rd.append(tick)
            else:
                b.r[eng] = tick
        for b in W:
            b.w = tick
            b.r = {}
            b.rd = []
        return tick

    def barrier(self):
        ticks = [t for t in self.last.values()] + [t for q in self.dtick for t in self.dtick[q] if t is not None]
        for e in ENG:
            waits = []
            seen = self.seen[e]
            for (s, v, se) in ticks:
                k = id(s)
                if seen.get(k, 0) >= v:
                    continue
                seen[k] = v
                waits.append((s, v))
            self.streams[e].append((waits, None, None))

    def emit(self, block):
        nc = self.nc

        def mk(name):
            def body(e):
                for waits, fn, tick in self.streams[name]:
                    for s, v in waits:
                        e.wait_ge(s, v)
                    if fn is not None:
                        ins = fn(e)
                        ins.then_inc(tick[0], 16 if tick[2] == 'dma' else 1)
            return body
        block.tensor(mk('pe'))
        block.scalar(mk('act'))
        block.vector(mk('dve'))
        block.gpsimd(mk('pool'))
        block.sync(mk('sp'))

    def mm(self, out, lhsT, rhs, start=True, stop=True, R=(), W=()):
        self.op('pe', lambda e: e.matmul(out, lhsT=lhsT, rhs=rhs, start=start, stop=stop), R, W)

    def tp(self, out, in_, ident, R=(), W=()):
        self.op('pe', lambda e: e.transpose(out, in_, ident), R, W)

    def act(self, out, in_, func, bias=None, scale=None, R=(), W=()):
        kw = {}
        if bias is not None:
            kw['bias'] = bias
        if scale is not None:
            kw['scale'] = scale
        self.op('act', lambda e: e.activation(out, in_, func, **kw), R, W)

    def ts(self, eng, out, in0, s1, s2, op0, op1=None, R=(), W=()):
        if op1 is None:
            self.op(eng, lambda e: e.tensor_scalar(out, in0, s1, None, op0), R, W)
        else:
            self.op(eng, lambda e: e.tensor_scalar(out, in0, s1, s2, op0, op1), R, W)

    def stt(self, out, in0, scalar, in1, op0, op1, R=(), W=(), accum=None):
        if accum is None:
            self.op('dve', lambda e: e.scalar_tensor_tensor(out, in0, scalar, in1, op0, op1), R, W)
        else:
            self.op('dve', lambda e: e.scalar_tensor_tensor(out, in0, scalar, in1, op0, op1, accum_out=accum), R, W)

    def tt(self, eng, out, in0, in1, op, R=(), W=()):
        self.op(eng, lambda e: e.tensor_tensor(out, in0, in1, op), R, W)

    def cp(self, eng, out, in_, R=(), W=()):
        if eng == 'act':
            self.op('act', lambda e: e.copy(out, in_), R, W)
        else:
            self.op(eng, lambda e: e.tensor_copy(out, in_), R, W)

    def memset(self, eng, ap, val, W=()):
        self.op(eng, lambda e: e.memset(ap, val), (), W)

    def dma(self, eng, out, in_, R=(), W=(), slow=False):
        if slow:
            self.op(eng, lambda e: e.dma_start(out=out, in_=in_, allow_slow_non_contiguous=True), R, W, dma=True)
        else:
            self.op(eng, lambda e: e.dma_start(out=out, in_=in_), R, W, dma=True)


class Arena:
    def __init__(self, ap, nbytes):
        self.ap = ap
        self.n = nbytes
        self.top = 0

    def alloc(self, shape, dt, parts=128):
        n = 1
        for s in shape:
            n *= s
        size = (n * _bytes(dt) + 63) // 64 * 64
        off = self.top
        self.top += size
        assert self.top <= self.n, "SBUF arena overflow %d > %d" % (self.top, self.n)
        v = self.ap[0:parts, off // 2:(off + n * _bytes(dt)) // 2]
        if dt != BF16:
            v = v.bitcast(dt)
        if len(shape) == 2:
            v = v.rearrange("p (a b) -> p a b", a=shape[0])
        elif len(shape) == 3:
            v = v.rearrange("p (a b c) -> p a b c", a=shape[0], b=shape[1])
        return v


def run_layers(rounds, interleave=True):
    gens = list(rounds)
    while gens:
        nxt = []
        for g in gens:
            try:
                next(g)
                nxt.append(g)
            except StopIteration:
                pass
        gens = nxt


def build(TSEG, NL, debug=False):
    T = 2 * TSEG
    NT = T // 128
    NTS = TSEG // 128
    NG = T // 512
    nc = bass.Bass("TRN2", target_bir_lowering=False)
    stack = ExitStack()

    def din(name, shape, dt=F32):
        return nc.dram_tensor(name, list(shape), dt, kind="ExternalInput").ap()

    skind = "ExternalOutput" if debug else "Internal"

    def dscr(name, shape, dt):
        return nc.dram_tensor(name, list(shape), dt, kind=skind).ap()

    x_in = din("x", [T, D])
    c2_in = din("c2", [2, D])
    cf_in = din("cf", [128, 1])
    w_ada = din("w_ada", [NL, D, 6 * D])
    b_ada = din("b_ada", [NL, 6 * D])
    w_in = din("w_in", [NL, D, IN_COLS])
    convw = din("convw", [NL, 128, 60])
    alog = din("alog", [NL, 8, 1])
    dtb = din("dtb", [NL, 8, 1])
    gnw = din("gnw", [NL, 128, 1])
    w_out = din("w_out", [NL, D, D])
    ln1g = din("ln1g", [NL, D])
    ln1b = din("ln1b", [NL, D])
    rw = din("rw", [NL, D, NE])
    rb = din("rb", [NL, NE])
    wgu = din("wgu", [NL, NE, D, 2 * DFF])
    bgu = din("bgu", [NL, 128, NE * 16])
    wd = din("wd", [NL, NE, DFF, D])
    bd = din("bd", [NL, NE, D])
    ln2g = din("ln2g", [NL, D])
    ln2b = din("ln2b", [NL, D])
    k_identf = din("k_identf", [128, 128])
    k_onesf = din("k_onesf", [128, 128])
    k_cm = din("k_cm", [128, 2 * 128])
    k_negm = din("k_negm", [128, 2 * 128])
    k_strict = din("k_strict", [128, 2 * 128])
    k_bm = din("k_bm", [128, 4 * 128])
    k_r = din("k_r", [128, 8 * 512])
    k_sel65 = din("k_sel65", [65, 64])
    k_sels = din("k_sels", [2, 256])
    y_out = nc.dram_tensor("y", [T, D], F32, kind="ExternalOutput").ap()

    winb = dscr("winb", [D, IN_COLS], BF16)
    woutb = dscr("woutb", [D, D], BF16)
    wgub = dscr("wgub", [NE, D, 2 * DFF], BF16)
    wdb = dscr("wdb", [NE, DFF, D], BF16)
    modrows = dscr("modrows", [2, 6 * D], F32)
    gdnT = dscr("gdnT", [2048, T], F32)
    abT = dscr("abT", [16, T], F32)
    attT = dscr("attT", [1536, T], BF16)
    mixT = dscr("mixT", [D, T], BF16)
    xmid = dscr("xmid", [T, D], F32)
    h2T = dscr("h2T", [D, T], BF16)
    x1 = dscr("x1", [T, D], F32)
    wrtD = dscr("wrtD", [T, NE], F32)

    ARENA_B = 200 * 1024
    arena_t = stack.enter_context(nc.sbuf_tensor("arena", [128, ARENA_B // 2], BF16))
    ps_t = [stack.enter_context(nc.psum_tensor("ps%d" % i, [128, 512], F32)) for i in range(8)]
    S = Sched(nc, stack)
    A = Arena(arena_t, ARENA_B)
    psb = bufs(8)
    pctr = [0]

    def psum():
        i = pctr[0] % 8
        pctr[0] += 1
        return ps_t[i], psb[i]

    identf = A.alloc([128], F32)
    identb = A.alloc([128], BF16)
    onesf = A.alloc([128], F32)
    onesb = A.alloc([128], BF16)
    cm = A.alloc([2, 128], F32)
    negm = A.alloc([2, 128], F32)
    strict = A.alloc([2, 128], F32)
    bm = A.alloc([4, 128], F32)
    sel65 = A.alloc([64], F32)
    cf = A.alloc([1], F32)
    epsln = A.alloc([1], F32)
    epsnm = A.alloc([1], F32)
    cb = Buf()
    S.dma('sp', identf, k_identf, W=[cb])
    S.dma('sp', onesf, k_onesf, W=[cb])
    S.dma('sp', cm, k_cm.rearrange("p (a b) -> p a b", a=2), W=[cb])
    S.dma('sp', negm, k_negm.rearrange("p (a b) -> p a b", a=2), W=[cb])
    S.dma('sp', strict, k_strict.rearrange("p (a b) -> p a b", a=2), W=[cb])
    S.dma('sp', bm, k_bm.rearrange("p (a b) -> p a b", a=4), W=[cb])
    S.dma('sp', sel65[0:65], k_sel65, W=[cb])
    S.dma('sp', cf, cf_in, W=[cb])
    S.barrier()
    S.cp('dve', identb, identf)
    S.cp('dve', onesb, onesf)
    S.memset('dve', epsln, LN_EPS)
    S.memset('dve', epsnm, NORM_EPS)
    S.barrier()
    persist_top = A.top

    def ln_stats(xt, xb, st6, mv, rstd, tb):
        S.op('dve', lambda e: e.bn_stats(st6[:, 0:6], xt[:, 0:512]), [xb], [tb])
        S.op('dve', lambda e: e.bn_stats(st6[:, 6:12], xt[:, 512:1024]), [xb], [tb])
        S.op('dve', lambda e: e.bn_aggr(mv, st6), [tb], [tb])
        S.act(rstd, mv[:, 1:2], AF.Sqrt, bias=epsln[:, 0:1], scale=1.0, R=[tb], W=[tb])
        S.op('dve', lambda e: e.reciprocal(rstd, rstd), [tb], [tb])

    for L in range(NL):
        x_src = x_in if L == 0 else x1
        x_dst = y_out if L == NL - 1 else x1
        A.top = persist_top
        NSL = 3
        stf = [A.alloc([2, 2048], F32) for _ in range(NSL)]
        stb = [A.alloc([2, 2048], BF16) for _ in range(NSL)]
        sfb = bufs(NSL)
        sbb = bufs(NSL)
        jobs = []
        for e in range(NE):
            for q in range(4):
                jobs.append((wgu[L, e, q * 256:(q + 1) * 256, :].rearrange("(a p) n -> p a n", p=128),
                             wgub[e, q * 256:(q + 1) * 256, :].rearrange("(a p) n -> p a n", p=128), (2, 2048)))
            for q in range(2):
                jobs.append((wd[L, e, q * 512:(q + 1) * 512, :].rearrange("(a p) n -> p a n", p=128),
                             wdb[e, q * 512:(q + 1) * 512, :].rearrange("(a p) n -> p a n", p=128), (4, 1024)))
        for q in range(8):
            jobs.append((w_in[L, q * 128:(q + 1) * 128, :].rearrange("(a p) n -> p a n", p=128),
                         winb[q * 128:(q + 1) * 128, :].rearrange("(a p) n -> p a n", p=128), (1, 3600)))
        for q in range(2):
            jobs.append((w_out[L, q * 512:(q + 1) * 512, :].rearrange("(a p) n -> p a n", p=128),
                         woutb[q * 512:(q + 1) * 512, :].rearrange("(a p) n -> p a n", p=128), (4, 1024)))
        ceng = ('act', 'dve', 'pool')
        for j, (src, dst, (a, n)) in enumerate(jobs):
            sl = j % NSL
            fv = stf[sl].rearrange("p a b -> p (a b)")[:, 0:a * n].rearrange("p (a b) -> p a b", a=a)
            bv = stb[sl].rearrange("p a b -> p (a b)")[:, 0:a * n].rearrange("p (a b) -> p a b", a=a)
            S.dma('sp', fv, src, W=[sfb[sl]])
            S.cp(ceng[j % 3], bv, fv, R=[sfb[sl]], W=[sbb[sl]])
            S.dma('pool' if j % 2 else 'sp', dst, bv, R=[sbb[sl]])
        S.barrier()

        A.top = persist_top
        cT = A.alloc([NKT, 2], F32)
        cTs = A.alloc([NKT, 2], F32)
        wa = [A.alloc([NKT, 512], F32) for _ in range(2)]
        wab = bufs(2)
        brow = A.alloc([6 * D], F32)
        mrow = A.alloc([6 * D], F32)
        b0 = Buf()
        for s in range(2):
            for kt in range(NKT):
                S.dma('sp', cT[:, kt, s:s + 1], c2_in[s:s + 1, kt * 128:(kt + 1) * 128].rearrange("s p -> p s"),
                      W=[b0], slow=True)
        S.dma('sp', brow[0:2], b_ada[L:L + 1, :].partition_broadcast(2).rearrange("p a n -> p (a n)"), W=[b0])
        S.act(cTs, cT, AF.Silu, R=[b0], W=[b0])
        for j in range(12):
            sl = j % 2
            S.dma('sp', wa[sl], w_ada[L, :, j * 512:(j + 1) * 512].rearrange("(kt p) n -> p kt n", p=128), W=[wab[sl]])
            pt, pb = psum()
            for kt in range(NKT):
                S.mm(pt[0:2, :], cTs[:, kt, :], wa[sl][:, kt, :], start=(kt == 0), stop=(kt == NKT - 1),
                     R=[wab[sl], b0], W=[pb])
            S.tt('dve', mrow[0:2, j * 512:(j + 1) * 512], pt[0:2, :], brow[0:2, j * 512:(j + 1) * 512], ALU.add,
                 R=[pb, b0], W=[b0])
        S.dma('sp', modrows, mrow[0:2], R=[b0])
        S.barrier()

        def load_mod_cols(dst, blk, tb):
            for s in range(2):
                S.dma('sp', dst[:, s, :], modrows[s, blk * D:(blk + 1) * D].rearrange("(kt p) -> p kt", p=128),
                      W=[tb], slow=True)

        def bcast_row(dst, row_ap, tb, eng='sp'):
            S.dma(eng, dst, row_ap.partition_broadcast(128).rearrange("p a n -> p (a n)"), W=[tb])

        A.top = persist_top
        wsb = A.alloc([NKT, IN_COLS], BF16)
        sh1 = A.alloc([2, 8], F32)
        sc1 = A.alloc([2, 8], F32)
        p1b = Buf()
        S.dma('sp', wsb, winb.rearrange("(kt p) n -> p kt n", p=128), W=[p1b])
        load_mod_cols(sh1, 0, p1b)
        load_mod_cols(sc1, 1, p1b)
        S.ts('dve', sc1, sc1, 1.0, None, ALU.add, R=[p1b], W=[p1b])
        S.barrier()
        NX = 3
        xt_ = [A.alloc([D], F32) for _ in range(NX)]
        xtb = bufs(NX)
        xn_ = [A.alloc([D], BF16) for _ in range(NX)]
        xnb = bufs(NX)
        st6 = [A.alloc([12], F32) for _ in range(NX)]
        mv_ = [A.alloc([2], F32) for _ in range(NX)]
        rs_ = [A.alloc([1], F32) for _ in range(NX)]
        stb_ = bufs(NX)
        hT = [A.alloc([NKT, 512], BF16) for _ in range(2)]
        hTb = bufs(2)
        NO = 4
        ostf = [A.alloc([512], F32) for _ in range(NO)]
        ostb = [A.alloc([512], BF16) for _ in range(NO)]
        ofb = bufs(NO)
        obb = bufs(NO)
        colchunks = [(i * 128, 128) for i in range(16)] + [(2048, 16)] + [(2064 + i * 128, 128) for i in range(12)]
        tcount = 0
        ocount = 0
        for g in range(NG):
            hs = g % 2
            for ti in range(4):
                t = g * 4 + ti
                seg = t // NTS
                sl = tcount % NX
                tcount += 1
                S.dma('sp', xt_[sl], x_src[t * 128:(t + 1) * 128, :], W=[xtb[sl]])
                ln_stats(xt_[sl], xtb[sl], st6[sl], mv_[sl], rs_[sl], stb_[sl])
                S.ts('dve', xn_[sl], xt_[sl], mv_[sl][:, 0:1], rs_[sl][:, 0:1], ALU.subtract, ALU.mult,
                     R=[xtb[sl], stb_[sl]], W=[xnb[sl]])
                pt, pb = psum()
                ptb = pt[:, :].bitcast(BF16).rearrange("p (a b) -> p a b", a=8)
                for kt in range(NKT):
                    S.tp(ptb[:, kt, :], xn_[sl][:, kt * 128:(kt + 1) * 128], identb, R=[xnb[sl]], W=[pb])
                for kt in range(NKT):
                    S.act(hT[hs][:, kt, ti * 128:(ti + 1) * 128], ptb[:, kt, :], AF.Identity,
                          bias=sh1[:, seg, kt:kt + 1], scale=sc1[:, seg, kt:kt + 1], R=[pb], W=[hTb[hs]])
            for ci, (c0, cn) in enumerate(colchunks):
                pt, pb = psum()
                for kt in range(NKT):
                    S.mm(pt[0:cn, :], wsb[:, kt, c0:c0 + cn], hT[hs][:, kt, :], start=(kt == 0), stop=(kt == NKT - 1),
                         R=[hTb[hs]], W=[pb])
                sl = ocount % NO
                ocount += 1
                ev = 'act' if ci % 2 else 'dve'
                cols = slice(g * 512, (g + 1) * 512)
                if ci < 16:
                    S.cp(ev, ostf[sl][0:cn], pt[0:cn, :], R=[pb], W=[ofb[sl]])
                    S.dma('pool' if ci % 2 else 'sp', gdnT[c0:c0 + cn, cols], ostf[sl][0:cn], R=[ofb[sl]])
                elif ci == 16:
                    S.cp(ev, ostf[sl][0:cn], pt[0:cn, :], R=[pb], W=[ofb[sl]])
                    S.dma('sp', abT[:, cols], ostf[sl][0:cn], R=[ofb[sl]])
                else:
                    r0 = (ci - 17) * 128
                    if ci < 21:
                        S.ts('dve', ostb[sl], pt[:, :], 0.125, None, ALU.mult, R=[pb], W=[obb[sl]])
                    else:
                        S.cp(ev, ostb[sl], pt[:, :], R=[pb], W=[obb[sl]])
                    S.dma('pool' if ci % 2 else 'sp', attT[r0:r0 + 128, cols], ostb[sl], R=[obb[sl]])
        S.barrier()

        A.top = persist_top
        qT = A.alloc([T], BF16)
        kp = A.alloc([T + 2 * PAD], BF16)
        vp = A.alloc([T + 2 * PAD], BF16)
        NVT = 80 if T >= 2048 else 80
        vaug = A.alloc([NVT, 2, 65], BF16)
        accT = A.alloc([2, T], F32)
        tmpf = [A.alloc([512], F32) for _ in range(2)]
        tmpfb = bufs(2)
        ptb_ = [A.alloc([512], BF16) for _ in range(2)]
        ptbb = bufs(2)
        recf = [A.alloc([512], F32) for _ in range(2)]
        recb = bufs(2)
        outb = [A.alloc([512], BF16) for _ in range(2)]
        outbb = bufs(2)
        ib = Buf()
        vab = bufs(NVT)
        rt = A.alloc([8, 512], F32)
        tmpb = A.alloc([512], F32)
        S.dma('sp', rt, k_r.rearrange("p (a b) -> p a b", a=8), W=[ib])
        alts = (2, 0, 3, 3)
        for i in range(4):
            S.tt('dve', tmpb, rt[:, 4 + i, :], rt[:, alts[i], :], ALU.subtract, R=[ib], W=[ib])
            S.stt(rt[:, 4 + i, :], tmpb, cf[:, 0:1], rt[:, alts[i], :], ALU.mult, ALU.add, R=[ib], W=[ib])
        S.memset('pool', vaug, 1.0, W=[ib])
        S.memset('pool', kp[:, 0:PAD], 0.0, W=[ib])
        S.memset('pool', kp[:, PAD + T:], 0.0, W=[ib])
        S.memset('pool', vp[:, 0:PAD], 0.0, W=[ib])
        S.memset('pool', vp[:, PAD + T:], 0.0, W=[ib])
        S.barrier()
        for hp in range(4):
            lb = Buf()
            S.dma('sp', qT, attT[hp * 128:(hp + 1) * 128, :], W=[lb])
            S.dma('sp', kp[:, PAD:PAD + T], attT[512 + hp * 128:512 + (hp + 1) * 128, :], W=[lb])
            S.dma('sp', vp[:, PAD:PAD + T], attT[1024 + hp * 128:1024 + (hp + 1) * 128, :], W=[lb])
            S.barrier()
            accb = [bufs(NG) for _ in range(2)]
            cnt = 0
            for pi, dil in enumerate(DILS):
                U = T // dil
                US = TSEG // dil
                nq = U // 128
                nqs = US // 128
                nkt = nq + 1
                vb = vab
                vi = 0
                for r in range(dil):
                    for k0 in range(0, nkt, 8):
                        kn = min(8, nkt - k0)
                        pt, pb = psum()
                        pv = pt[:, :].bitcast(BF16).rearrange("p (a b) -> p a b", a=8)
                        for kk in range(kn):
                            kt = k0 + kk
                            st = PAD + r + dil * (128 * kt - 64)
                            S.tp(pv[:, kk, :], vp[:, st:st + dil * 127 + 1:dil], identb, W=[pb])
                        sl0 = r * nkt + k0
                        S.cp('act' if vi % 2 else 'dve',
                             vaug[:, sl0:sl0 + kn, :, 0:64],
                             pv[:, 0:kn, :].rearrange("p a (h d) -> p a h d", h=2),
                             R=[pb], W=vb[sl0:sl0 + kn])
                        vi += 1
                for h in range(2):
                    h0 = h * 64
                    chp = dil * (2.0 ** (-(2 * hp + h + 1)))
                    for r in range(dil):
                        for qp in range(0, nq, 4):
                            po, pob = psum()
                            for q2 in (0, 2):
                                qa = qp + q2
                                segq = qa // nqs
                                la = qa % nqs
                                first = (la == 0)
                                lastp = (la + 2 == nqs)
                                if nqs == 2:
                                    ridx = 6 if segq == 0 else 7
                                    if segq == 0:
                                        ridx = 6
                                    else:
                                        ridx = 7
                                elif first:
                                    ridx = 0 if segq == 0 else 5
                                elif lastp:
                                    ridx = 4 if segq == 0 else 2
                                else:
                                    ridx = 1
                                pt, pb = psum()
                                for qq in range(2):
                                    q = qa + qq
                                    qs = r + dil * 128 * q
                                    for half in range(2):
                                        ks = PAD + r + dil * (128 * (q + half) - 64)
                                        S.mm(pt[:, qq * 256 + half * 128: qq * 256 + (half + 1) * 128],
                                             kp[h0:h0 + 64, ks:ks + dil * 127 + 1:dil],
                                             qT[h0:h0 + 64, qs:qs + dil * 127 + 1:dil], W=[pb])
                                sl = cnt % 2
                                cnt += 1
                                S.stt(tmpf[sl], rt[:, ridx, :], chp, pt[:, :], ALU.mult, ALU.add,
                                      R=[pb], W=[tmpfb[sl]])
                                S.act(ptb_[sl], tmpf[sl], AF.Exp, R=[tmpfb[sl]], W=[ptbb[sl]])
                                for qq in range(2):
                                    q = qa + qq
                                    for half in range(2):
                                        vt = r * nkt + q + half
                                        S.mm(po[0:65, (q2 + qq) * 128:(q2 + qq + 1) * 128],
                                             vaug[:, vt, h, :],
                                             ptb_[sl][:, qq * 256 + half * 128: qq * 256 + (half + 1) * 128],
                                             start=(half == 0), stop=(half == 1),
                                             R=[ptbb[sl], vb[vt]], W=[pob])
                            ts0 = r + dil * 128 * qp
                            dst = accT[0:65, h, ts0:ts0 + dil * 511 + 1:dil]
                            glo = ts0 // 512
                            ghi = (ts0 + dil * 512 - 1) // 512
                            ab_ = accb[h][glo:ghi + 1]
                            if pi == 0:
                                S.cp('act', dst, po[0:65, :], R=[pob], W=ab_)
                            else:
                                S.tt('dve', dst, po[0:65, :], dst, ALU.add, R=[pob] + ab_, W=ab_)
            for h in range(2):
                for g in range(NG):
                    cols = slice(g * 512, (g + 1) * 512)
                    pt, pb = psum()
                    S.mm(pt[0:64, :], sel65[0:65, :], accT[0:65, h, cols], R=[accb[h][g]], W=[pb])
                    sl = g % 2
                    S.op('dve', lambda e, o=recf[sl][0:64], i=pt[0:64, :]: e.reciprocal(o, i), [pb], [recb[sl]])
                    S.tt('pool', outb[sl][0:64], accT[0:64, h, cols], recf[sl][0:64], ALU.mult,
                         R=[recb[sl], accb[h][g]], W=[outbb[sl]])
                    r0 = 512 + (2 * hp + h) * 64
                    S.dma('sp', mixT[r0:r0 + 64, cols], outb[sl][0:64], R=[outbb[sl]])
            S.barrier()

        A.top = persist_top
        gtok = A.alloc([NT, 8], F32)
        btok = A.alloc([NT, 8], F32)
        gcF = A.alloc([NT, 8], F32)
        gcB = A.alloc([NT, 8], F32)
        gtot = A.alloc([NT, 8], F32)
        negegcF = A.alloc([NT, 8], F32)
        negegcB = A.alloc([NT, 8], F32)
        kdecF = A.alloc([NT, 8], F32)
        kdecB = A.alloc([NT, 8], F32)
        glast = A.alloc([NT, 8], F32)
        nbeta = A.alloc([NT, 8], F32)
        dt8 = A.alloc([1], F32)
        na8 = A.alloc([1], F32)
        cw = A.alloc([60], F32)
        gw = A.alloc([1], F32)
        gdn_top = A.top
        ga = A.alloc([T], F32)
        gbta = A.alloc([T], F32)
        pb0 = Buf()
        S.dma('sp', ga[0:8], abT[0:8, :], W=[pb0])
        S.dma('sp', gbta[0:8], abT[8:16, :], W=[pb0])
        S.dma('sp', dt8[0:8], dtb[L], W=[pb0])
        S.dma('sp', na8[0:8], alog[L], W=[pb0])
        S.dma('sp', cw, convw[L], W=[pb0])
        S.dma('sp', gw, gnw[L], W=[pb0])
        S.act(na8[0:8], na8[0:8], AF.Exp, R=[pb0], W=[pb0])
        S.ts('dve', na8[0:8], na8[0:8], -1.0, None, ALU.mult, R=[pb0], W=[pb0])
        S.act(ga[0:8], ga[0:8], AF.Exp, bias=dt8[0:8, 0:1], scale=1.0, R=[pb0], W=[pb0])
        S.ts('dve', ga[0:8], ga[0:8], 1.0, None, ALU.add, R=[pb0], W=[pb0])
        S.act(ga[0:8], ga[0:8], AF.Ln, R=[pb0], W=[pb0])
        S.ts('dve', ga[0:8], ga[0:8], na8[0:8, 0:1], None, ALU.mult, R=[pb0], W=[pb0])
        S.act(gbta[0:8], gbta[0:8], AF.Sigmoid, R=[pb0], W=[pb0])
        for (srcr, dsttok) in ((ga, gtok), (gbta, btok)):
            for t0 in range(0, NT, 64):
                tn = min(64, NT - t0)
                pt, pb = psum()
                pv = pt[:, :].rearrange("p (a b) -> p a b", b=8)
                for tt_ in range(tn):
                    t = t0 + tt_
                    S.tp(pv[:, tt_, :], srcr[0:8, t * 128:(t + 1) * 128], identf[0:8, 0:8], R=[pb0], W=[pb])
                S.cp('dve', dsttok[:, t0:t0 + tn, :], pv[:, 0:tn, :], R=[pb], W=[pb0])
        for t0 in range(0, NT, 64):
            tn = min(64, NT - t0)
            rhs = gtok[:, t0:t0 + tn, :].rearrange("p a b -> p (a b)")
            for (dstt, lhs) in ((gcF, cm[:, 0, :]), (gcB, cm[:, 1, :]), (gtot, onesf)):
                pt, pb = psum()
                S.mm(pt[:, 0:tn * 8], lhs, rhs, R=[pb0], W=[pb])
                S.cp('dve', dstt[:, t0:t0 + tn, :].rearrange("p a b -> p (a b)"), pt[:, 0:tn * 8], R=[pb], W=[pb0])
        for (gc_, ne_, kd_) in ((gcF, negegcF, kdecF), (gcB, negegcB, kdecB)):
            S.act(ne_, gc_, AF.Exp, R=[pb0], W=[pb0])
            S.ts('dve', ne_, ne_, -1.0, None, ALU.mult, R=[pb0], W=[pb0])
            S.tt('dve', kd_, gtot, gc_, ALU.subtract, R=[pb0], W=[pb0])
            S.act(kd_, kd_, AF.Exp, R=[pb0], W=[pb0])
        S.act(glast, gtot, AF.Exp, R=[pb0], W=[pb0])
        S.ts('dve', nbeta, btok, -1.0, None, ALU.mult, R=[pb0], W=[pb0])
        S.barrier()
        for hd in range(4):
            A.top = gdn_top
            qTg = A.alloc([T], BF16)
            kTg = A.alloc([T], BF16)
            ktok = A.alloc([NT, 128], BF16)
            vtok = A.alloc([NT, 128], BF16)
            oacc = A.alloc([NT, 128], F32)
            head_top = A.top
            CH = 1024 if T >= 1024 else T
            NCH = T // CH
            xr = [A.alloc([CH + 4], F32) for _ in range(2)]
            xrb = bufs(2)
            cacc = [A.alloc([CH], F32) for _ in range(2)]
            caccb = bufs(2)
            ysl = [A.alloc([CH], F32) for _ in range(2)]
            yslb = bufs(2)
            sq = A.alloc([CH], F32)
            sqb = Buf()
            rn = A.alloc([CH], F32)
            rnb = Buf()
            vTb = A.alloc([CH], BF16)
            vTbb = Buf()
            chb = bufs(NCH)
            jc = 0
            for which in range(3):
                row0 = which * 512 + hd * 128
                for c in range(NCH):
                    t0 = c * CH
                    sl = jc % 2
                    jc += 1
                    lo = t0 - 2
                    hi = t0 + CH + 2
                    X = xr[sl]
                    if lo < 0:
                        S.memset('pool', X[:, 0:2], 0.0, W=[xrb[sl]])
                        S.dma('sp', X[:, 2:CH + 2], gdnT[row0:row0 + 128, t0:t0 + CH], W=[xrb[sl]])
                        if hi > T:
                            S.memset('pool', X[:, CH + 2:CH + 4], 0.0, W=[xrb[sl]])
                        else:
                            S.dma('sp', X[:, CH + 2:CH + 4], gdnT[row0:row0 + 128, t0 + CH:t0 + CH + 2], W=[xrb[sl]])
                    elif hi > T:
                        S.dma('sp', X[:, 0:CH + 2], gdnT[row0:row0 + 128, lo:t0 + CH], W=[xrb[sl]])
                        S.memset('pool', X[:, CH + 2:CH + 4], 0.0, W=[xrb[sl]])
                    else:
                        S.dma('sp', X[:, :], gdnT[row0:row0 + 128, lo:hi], W=[xrb[sl]])
                    if t0 == TSEG:
                        S.ts('pool', X[:, 0:2], X[:, 0:2], cf[:, 0:1], None, ALU.mult, R=[xrb[sl]], W=[xrb[sl]])
                    if t0 + CH == TSEG:
                        S.ts('pool', X[:, CH + 2:CH + 4], X[:, CH + 2:CH + 4], cf[:, 0:1], None, ALU.mult,
                             R=[xrb[sl]], W=[xrb[sl]])
                    wbase = (which * 4 + hd) * 5
                    S.ts('dve', cacc[sl], X[:, 0:CH], cw[:, wbase:wbase + 1], None, ALU.mult,
                         R=[xrb[sl], pb0], W=[caccb[sl]])
                    for j in range(1, 5):
                        S.stt(cacc[sl], X[:, j:j + CH], cw[:, wbase + j:wbase + j + 1], cacc[sl], ALU.mult, ALU.add,
                              R=[xrb[sl], caccb[sl]], W=[caccb[sl]])
                    S.act(ysl[sl], cacc[sl], AF.Silu, R=[caccb[sl]], W=[yslb[sl]])
                    if which < 2:
                        S.act(sq, ysl[sl], AF.Square, R=[yslb[sl]], W=[sqb])
                        for hf in range(CH // 512):
                            pt, pb = psum()
                            S.mm(pt[:, :], onesf, sq[:, hf * 512:(hf + 1) * 512], R=[sqb], W=[pb])
                            S.act(rn[:, hf * 512:(hf + 1) * 512], pt[:, :], AF.Sqrt, bias=epsnm[:, 0:1], scale=1.0,
                                  R=[pb], W=[rnb])
                        S.op('dve', lambda e, o=rn: e.reciprocal(o, o), [rnb], [rnb])
                        dstT = qTg if which == 0 else kTg
                        S.stt(dstT[:, t0:t0 + CH], ysl[sl], (128.0 ** -0.5) if which == 0 else 1.0, rn,
                              ALU.mult, ALU.mult, R=[yslb[sl], rnb], W=[chb[c]])
                        srcT = dstT
                        sb_ = chb[c]
                    else:
                        S.cp('pool', vTb, ysl[sl], R=[yslb[sl]], W=[vTbb])
                        srcT = None
                    if which >= 1:
                        dtok = ktok if which == 1 else vtok
                        for k0 in range(0, CH // 128, 8):
                            pt, pb = psum()
                            pv = pt[:, :].bitcast(BF16).rearrange("p (a b) -> p a b", a=8)
                            for kk in range(8):
                                cc0 = (k0 + kk) * 128
                                if which == 1:
                                    S.tp(pv[:, kk, :], kTg[:, t0 + cc0:t0 + cc0 + 128], identb, R=[chb[c]], W=[pb])
                                else:
                                    S.tp(pv[:, kk, :], vTb[:, cc0:cc0 + 128], identb, R=[vTbb], W=[pb])
                            tl0 = t0 // 128 + k0
                            S.cp('act', dtok[:, tl0:tl0 + 8, :], pv, R=[pb], W=[chb[c]])
            S.barrier()
            A.top = head_top
            RING = 3
            TTr = [[A.alloc([128], BF16) for _ in range(RING)] for _ in range(2)]
            ITr = [[A.alloc([128], BF16) for _ in range(RING)] for _ in range(2)]
            QDr = [[A.alloc([128], BF16) for _ in range(RING)] for _ in range(2)]
            KDr = [[A.alloc([128], BF16) for _ in range(RING)] for _ in range(2)]
            rb_ = [[Buf() for _ in range(RING)] for _ in range(2)]
            dgt = [A.alloc([128], F32) for _ in range(2)]
            Et = [A.alloc([128], F32) for _ in range(2)]
            DTt = [A.alloc([128], F32) for _ in range(2)]
            DTs = [A.alloc([128], F32) for _ in range(2)]
            EGB = [A.alloc([128], BF16) for _ in range(2)]
            Pm = [A.alloc([2, 128], F32) for _ in range(2)]
            Nd = [A.alloc([2, 128], F32) for _ in range(2)]
            aD = [A.alloc([2, 128], F32) for _ in range(2)]
            No = [A.alloc([2, 128], F32) for _ in range(2)]
            Ww = [A.alloc([2, 128], F32) for _ in range(2)]
            wkb = [Buf() for _ in range(2)]
            Sf = [A.alloc([128], F32) for _ in range(2)]
            Sb = [A.alloc([128], BF16) for _ in range(2)]
            Sbuf = bufs(2)
            Xt = [A.alloc([128], BF16) for _ in range(2)]
            vnt = [A.alloc([128], BF16) for _ in range(2)]
            xb_ = bufs(2)
            ob = bufs(NT)
            visited = [False] * NT
            for d_ in range(2):
                S.memset('pool', Sf[d_], 0.0, W=[Sbuf[d_]])
                S.memset('pool', Sb[d_], 0.0, W=[Sbuf[d_]])
            order = [list(range(NT)), list(range(NT - 1, -1, -1))]

            def pre(d_, si):
                t = order[d_][si]
                ci = d_ * 4 + hd
                gc_ = (gcF, gcB)[d_]
                kd_ = (kdecF, kdecB)[d_]
                slot = si % RING
                rbuf = rb_[d_][slot]
                wb = wkb[d_]
                tc = slice(t * 128, (t + 1) * 128)
                gcol = gc_[:, t, ci:ci + 1]
                S.ts('pool', dgt[d_], identf, gcol, None, ALU.mult, W=[wb])
                pA, pAb = ps_t[4 + 2 * d_], psb[4 + 2 * d_]
                S.mm(pA[:, 0:128], onesf, dgt[d_], R=[wb], W=[pAb])
                S.mm(pA[:, 128:256], kTg[:, tc], kTg[:, tc], W=[pAb])
                S.mm(pA[:, 256:384], kTg[:, tc], qTg[:, tc], W=[pAb])
                yield
                S.stt(Et[d_], pA[:, 0:128], gcol, negm[:, d_, :], ALU.subtract, ALU.add, R=[pAb], W=[wb])
                S.act(EGB[d_], pA[:, 0:128], AF.Exp, R=[pAb], W=[wb])
                S.act(DTt[d_], Et[d_], AF.Exp, R=[wb], W=[wb])
                S.tt('pool', QDr[d_][slot], qTg[:, tc], EGB[d_], ALU.mult, R=[wb], W=[rbuf])
                S.ts('pool', KDr[d_][slot], ktok[:, t, :], kd_[:, t, ci:ci + 1], None, ALU.mult, W=[rbuf])
                S.tt('pool', DTs[d_], DTt[d_], strict[:, d_, :], ALU.mult, R=[wb], W=[wb])
                yield
                S.stt(Pm[d_][:, 0, :], pA[:, 128:256], nbeta[:, t, ci:ci + 1], DTs[d_], ALU.mult, ALU.mult,
                      R=[pAb, wb], W=[wb])
                S.tt('dve', ITr[d_][slot], pA[:, 256:384], DTt[d_], ALU.mult, R=[pAb, wb], W=[rbuf])
                pB, pBb = ps_t[5 + 2 * d_], psb[5 + 2 * d_]
                pBv = pB
                S.tp(pBv[:, 0:128], Pm[d_][:, 0, :], identf, R=[wb], W=[pBb])
                yield
                S.cp('act', Pm[d_][:, 1, :], pBv[:, 0:128], R=[pBb], W=[wb])
                yield
                PB = ps_t[5 + 2 * d_]
                PBb = psb[5 + 2 * d_]
                nd = Nd[d_]
                ad = aD[d_]
                S.tt('pool', nd[:, 0, :], Pm[d_][:, 0, :], bm[:, 0, :], ALU.mult, R=[wb], W=[wb])
                S.tt('pool', nd[:, 1, :], Pm[d_][:, 1, :], bm[:, 0, :], ALU.mult, R=[wb], W=[wb])
                S.tt('pool', ad[:, 0, :], nd[:, 0, :], identf, ALU.add, R=[wb], W=[wb])
                S.tt('pool', ad[:, 1, :], nd[:, 1, :], identf, ALU.add, R=[wb], W=[wb])
                yield
                for lvl in range(3):
                    S.mm(PB[:, 0:128], nd[:, 1, :], nd[:, 0, :], R=[wb], W=[PBb])
                    S.mm(PB[:, 128:256], nd[:, 0, :], nd[:, 1, :], R=[wb], W=[PBb])
                    yield
                    S.cp('act', nd.rearrange("p a b -> p (a b)"), PB[:, 0:256], R=[PBb], W=[wb])
                    S.mm(PB[:, 256:384], nd[:, 1, :], ad[:, 0, :], R=[wb], W=[PBb])
                    S.mm(PB[:, 384:512], nd[:, 0, :], ad[:, 1, :], R=[wb], W=[PBb])
                    yield
                    adf = ad.rearrange("p a b -> p (a b)")
                    S.tt('dve', adf, PB[:, 256:512], adf, ALU.add, R=[PBb, wb], W=[wb])
                    yield
                no = No[d_]
                ww = Ww[d_]
                for mi in (1, 2, 3):
                    S.tt('pool', no[:, 0, :], Pm[d_][:, 0, :], bm[:, mi, :], ALU.mult, R=[wb], W=[wb])
                    S.tt('pool', no[:, 1, :], Pm[d_][:, 1, :], bm[:, mi, :], ALU.mult, R=[wb], W=[wb])
                    S.mm(PB[:, 0:128], no[:, 1, :], ad[:, 0, :], R=[wb], W=[PBb])
                    S.mm(PB[:, 128:256], no[:, 0, :], ad[:, 1, :], R=[wb], W=[PBb])
                    yield
                    S.cp('act', ww.rearrange("p a b -> p (a b)"), PB[:, 0:256], R=[PBb], W=[wb])
                    S.mm(PB[:, 256:384], ad[:, 1, :], ww[:, 0, :], R=[wb], W=[PBb])
                    if mi < 3:
                        S.mm(PB[:, 384:512], ad[:, 0, :], ww[:, 1, :], R=[wb], W=[PBb])
                    yield
                    if mi < 3:
                        adf = ad.rearrange("p a b -> p (a b)")
                        S.tt('dve', adf, PB[:, 256:512], adf, ALU.add, R=[PBb, wb], W=[wb])
                    else:
                        S.tt('dve', TTr[d_][slot], PB[:, 256:384], ad[:, 0, :], ALU.add, R=[PBb, wb], W=[rbuf])
                    yield

            def scan(d_, si):
                t = order[d_][si]
                ci = d_ * 4 + hd
                ne_ = (negegcF, negegcB)[d_]
                slot = si % RING
                rbuf = rb_[d_][slot]
                tc = slice(t * 128, (t + 1) * 128)
                pA, pAb = ps_t[2 * d_], psb[2 * d_]
                pO, pOb = ps_t[2 * d_ + 1], psb[2 * d_ + 1]
                S.mm(pA[:, 0:128], kTg[:, tc], Sb[d_], R=[Sbuf[d_]], W=[pAb])
                S.mm(pO[:, 0:128], QDr[d_][slot], Sb[d_], start=True, stop=False, R=[Sbuf[d_], rbuf], W=[pOb])
                yield
                S.stt(Xt[d_], pA[:, 0:128], ne_[:, t, ci:ci + 1], vtok[:, t, :], ALU.mult, ALU.add,
                      R=[pAb], W=[xb_[d_]])
                yield
                S.mm(pA[:, 128:256], TTr[d_][slot], Xt[d_], R=[xb_[d_], rbuf], W=[pAb])
                yield
                S.act(vnt[d_], pA[:, 128:256], AF.Copy, scale=btok[:, t, ci:ci + 1], R=[pAb], W=[xb_[d_]])
                yield
                S.mm(pO[:, 0:128], ITr[d_][slot], vnt[d_], start=False, stop=True, R=[xb_[d_], rbuf], W=[pOb])
                S.mm(pA[:, 256:384], KDr[d_][slot], vnt[d_], R=[xb_[d_], rbuf], W=[pAb])
                yield
                if not visited[t]:
                    visited[t] = True
                    S.cp('act', oacc[:, t, :], pO[:, 0:128], R=[pOb], W=[ob[t]])
                else:
                    S.tt('dve', oacc[:, t, :], pO[:, 0:128], oacc[:, t, :], ALU.add, R=[pOb, ob[t]], W=[ob[t]])
                S.stt(Sf[d_], Sf[d_], glast[:, t, ci:ci + 1], pA[:, 256:384], ALU.mult, ALU.add,
                      R=[pAb, Sbuf[d_]], W=[Sbuf[d_]])
                nxt = si + 1
                if nxt < NT:
                    tn_ = order[d_][nxt]
                    if (tn_ // NTS) != (t // NTS):
                        S.ts('dve', Sf[d_], Sf[d_], cf[:, 0:1], None, ALU.mult, R=[Sbuf[d_]], W=[Sbuf[d_]])
                S.cp('pool', Sb[d_], Sf[d_], R=[Sbuf[d_]], W=[Sbuf[d_]])
                yield

            for si in range(NT + 1):
                gens = []
                if si < NT:
                    gens += [pre(0, si), pre(1, si)]
                if si >= 1:
                    gens += [scan(0, si - 1), scan(1, si - 1)]
                run_layers(gens)
            sqs = A.alloc([NT], F32)
            fb = Buf()
            zt = [A.alloc([512], F32) for _ in range(2)]
            ztb = bufs(2)
            onb = [A.alloc([128], BF16) for _ in range(2)]
            onbb = bufs(2)
            og = [A.alloc([512], BF16) for _ in range(2)]
            ogb = bufs(2)
            for t in range(NT):
                S.act(Et[0], oacc[:, t, :], AF.Square, R=[ob[t]], W=[wkb[0]])
                S.op('dve', lambda e, o=sqs[:, t:t + 1], i=Et[0]: e.tensor_reduce(o, i, AX.X, ALU.add), [wkb[0]], [fb])
            S.act(sqs, sqs, AF.Sqrt, bias=epsnm[:, 0:1], scale=1.0 / 128.0, R=[fb], W=[fb])
            S.op('dve', lambda e: e.reciprocal(sqs, sqs), [fb], [fb])
            for g in range(NG):
                sl = g % 2
                S.dma('sp', zt[sl], gdnT[1536 + hd * 128:1536 + (hd + 1) * 128, g * 512:(g + 1) * 512], W=[ztb[sl]])
                S.act(zt[sl], zt[sl], AF.Silu, R=[ztb[sl]], W=[ztb[sl]])
                pt, pb = psum()
                pv = pt[:, :].bitcast(BF16).rearrange("p (a b) -> p a b", a=8)
                for ti in range(4):
                    t = g * 4 + ti
                    s2 = ti % 2
                    S.ts('pool', onb[s2], oacc[:, t, :], sqs[:, t:t + 1], None, ALU.mult, R=[ob[t], fb], W=[onbb[s2]])
                    S.tp(pv[:, ti, :], onb[s2], identb, R=[onbb[s2]], W=[pb])
                S.stt(og[sl], pv[:, 0:4, :].rearrange("p a b -> p (a b)"), gw[:, 0:1], zt[sl], ALU.mult, ALU.mult,
                      R=[pb, ztb[sl], pb0], W=[ogb[sl]])
                S.dma('sp', mixT[hd * 128:(hd + 1) * 128, g * 512:(g + 1) * 512], og[sl], R=[ogb[sl]])
            S.barrier()

        A.top = persist_top
        wo = A.alloc([NKT, D], BF16)
        g1b = A.alloc([2, D], F32)
        l1g = A.alloc([D], F32)
        l1b = A.alloc([D], F32)
        sh2 = A.alloc([2, 8], F32)
        sc2 = A.alloc([2, 8], F32)
        rwf = A.alloc([NKT, NE], F32)
        rbb = A.alloc([NE], F32)
        p3 = Buf()
        S.dma('sp', wo, woutb.rearrange("(kt p) n -> p kt n", p=128), W=[p3])
        for s in range(2):
            bcast_row(g1b[:, s, :], modrows[s:s + 1, 2 * D:3 * D], p3)
        bcast_row(l1g, ln1g[L:L + 1, :], p3)
        bcast_row(l1b, ln1b[L:L + 1, :], p3)
        bcast_row(rbb, rb[L:L + 1, :], p3)
        load_mod_cols(sh2, 3, p3)
        load_mod_cols(sc2, 4, p3)
        S.ts('dve', sc2, sc2, 1.0, None, ALU.add, R=[p3], W=[p3])
        S.dma('sp', rwf, rw[L].rearrange("(kt p) n -> p kt n", p=128), W=[p3])
        S.barrier()
        NP = 2
        mx = [A.alloc([NKT, 128], BF16) for _ in range(NP)]
        mxb = bufs(NP)
        xt3 = [A.alloc([D], F32) for _ in range(NP)]
        xt3b = bufs(NP)
        yt3 = [A.alloc([D], F32) for _ in range(NP)]
        yt3b = bufs(NP)
        xm3 = [A.alloc([D], F32) for _ in range(NP)]
        xm3b = bufs(NP)
        xn3 = [A.alloc([D], F32) for _ in range(NP)]
        xn3b = bufs(NP)
        h2f = [A.alloc([NKT, 128], F32) for _ in range(NP)]
        h2fb = bufs(NP)
        h2b = [A.alloc([NKT, 128], BF16) for _ in range(NP)]
        h2bb = bufs(NP)
        st63 = [A.alloc([12], F32) for _ in range(NP)]
        mv3 = [A.alloc([2], F32) for _ in range(NP)]
        rs3 = [A.alloc([1], F32) for _ in range(NP)]
        stb3 = bufs(NP)
        lg = [A.alloc([NE], F32) for _ in range(NP)]
        t8 = [A.alloc([8], F32) for _ in range(NP)]
        mk3 = [A.alloc([NE], F32) for _ in range(NP)]
        ex3 = [A.alloc([NE], F32) for _ in range(NP)]
        sm3 = [A.alloc([1], F32) for _ in range(NP)]
        nm3 = [A.alloc([1], F32) for _ in range(NP)]
        rtb3 = bufs(NP)
        wrb = Buf()
        for t in range(NT):
            sl = t % NP
            seg = t // NTS
            rows = slice(t * 128, (t + 1) * 128)
            S.dma('sp', mx[sl], mixT[:, rows].rearrange("(kt p) n -> p kt n", p=128), W=[mxb[sl]])
            S.dma('pool', xt3[sl], x_src[rows, :], W=[xt3b[sl]])
            for hf in range(2):
                pt, pb = psum()
                for kt in range(NKT):
                    S.mm(pt[:, :], mx[sl][:, kt, :], wo[:, kt, hf * 512:(hf + 1) * 512], start=(kt == 0),
                         stop=(kt == NKT - 1), R=[mxb[sl]], W=[pb])
                S.tt('dve', yt3[sl][:, hf * 512:(hf + 1) * 512], pt[:, :], g1b[:, seg, hf * 512:(hf + 1) * 512],
                     ALU.mult, R=[pb], W=[yt3b[sl]])
            S.stt(yt3[sl], xt3[sl], ALPHA, yt3[sl], ALU.mult, ALU.add, R=[xt3b[sl], yt3b[sl]], W=[yt3b[sl]])
            ln_stats(yt3[sl], yt3b[sl], st63[sl], mv3[sl], rs3[sl], stb3[sl])
            S.ts('dve', yt3[sl], yt3[sl], mv3[sl][:, 0:1], rs3[sl][:, 0:1], ALU.subtract, ALU.mult,
                 R=[yt3b[sl], stb3[sl]], W=[yt3b[sl]])
            S.tt('pool', yt3[sl], yt3[sl], l1g, ALU.mult, R=[yt3b[sl]], W=[yt3b[sl]])
            S.tt('pool', xm3[sl], yt3[sl], l1b, ALU.add, R=[yt3b[sl]], W=[xm3b[sl]])
            S.dma('sp', xmid[rows, :], xm3[sl], R=[xm3b[sl]])
            ln_stats(xm3[sl], xm3b[sl], st63[sl], mv3[sl], rs3[sl], stb3[sl])
            S.ts('dve', xn3[sl], xm3[sl], mv3[sl][:, 0:1], rs3[sl][:, 0:1], ALU.subtract, ALU.mult,
                 R=[xm3b[sl], stb3[sl]], W=[xn3b[sl]])
            for hf in range(2):
                pt, pb = psum()
                for k4 in range(4):
                    kt = hf * 4 + k4
                    S.tp(pt[:, k4 * 128:(k4 + 1) * 128], xn3[sl][:, kt * 128:(kt + 1) * 128], identf,
                         R=[xn3b[sl]], W=[pb])
                for k4 in range(4):
                    kt = hf * 4 + k4
                    S.act(h2f[sl][:, kt, :], pt[:, k4 * 128:(k4 + 1) * 128], AF.Identity,
                          bias=sh2[:, seg, kt:kt + 1], scale=sc2[:, seg, kt:kt + 1], R=[pb], W=[h2fb[sl]])
            S.cp('pool', h2b[sl], h2f[sl], R=[h2fb[sl]], W=[h2bb[sl]])
            S.dma('sp', h2T[:, rows].rearrange("(kt p) n -> p kt n", p=128), h2b[sl], R=[h2bb[sl]])
            pt, pb = psum()
            for kt in range(NKT):
                S.mm(pt[:, 0:NE], h2f[sl][:, kt, :], rwf[:, kt, :], start=(kt == 0), stop=(kt == NKT - 1),
                     R=[h2fb[sl]], W=[pb])
            rb3 = rtb3[sl]
            S.tt('dve', lg[sl], pt[:, 0:NE], rbb, ALU.add, R=[pb], W=[rb3])
            S.op('dve', lambda e, o=t8[sl], i=lg[sl]: e.max(o, i), [rb3], [rb3])
            S.ts('dve', mk3[sl], lg[sl], t8[sl][:, 3:4], None, ALU.is_ge, R=[rb3], W=[rb3])
            S.ts('dve', nm3[sl], t8[sl][:, 0:1], -1.0, None, ALU.mult, R=[rb3], W=[rb3])
            S.act(ex3[sl], lg[sl], AF.Exp, bias=nm3[sl][:, 0:1], scale=1.0, R=[rb3], W=[rb3])
            S.tt('dve', ex3[sl], ex3[sl], mk3[sl], ALU.mult, R=[rb3], W=[rb3])
            S.op('dve', lambda e, o=sm3[sl], i=ex3[sl]: e.tensor_reduce(o, i, AX.X, ALU.add), [rb3], [rb3])
            S.op('dve', lambda e, o=sm3[sl]: e.reciprocal(o, o), [rb3], [rb3])
            S.ts('dve', ex3[sl], ex3[sl], sm3[sl][:, 0:1], None, ALU.mult, R=[rb3], W=[rb3])
            S.dma('sp', wrtD[rows, :], ex3[sl], R=[rb3])
        S.barrier()

        A.top = persist_top
        TC = 1024 if T >= 1024 else T
        NTC = TC // 128
        NCK = T // TC
        hch = A.alloc([NKT, TC], BF16)
        acc4 = A.alloc([NTC, D], F32)
        actT = A.alloc([8, TC], BF16)
        wg_sb = A.alloc([NKT, 2 * DFF], BF16)
        wd_sb = A.alloc([8, D], BF16)
        bgu_sb = A.alloc([NE, 16], F32)
        bd_sb = A.alloc([D], F32)
        g2b = A.alloc([2, D], F32)
        l2g = A.alloc([D], F32)
        l2b = A.alloc([D], F32)
        wrtT = A.alloc([NTC, 128], F32)
        wrt = A.alloc([NTC, NE], F32)
        wrb4 = Buf()
        p4 = Buf()
        S.dma('sp', bgu_sb, bgu[L].rearrange("p (e c) -> p e c", e=NE), W=[p4])
        S.dma('sp', bd_sb[0:NE], bd[L], W=[p4])
        for s in range(2):
            bcast_row(g2b[:, s, :], modrows[s:s + 1, 5 * D:6 * D], p4)
        bcast_row(l2g, ln2g[L:L + 1, :], p4)
        bcast_row(l2b, ln2b[L:L + 1, :], p4)
        S.barrier()
        NQ = 2
        gt_ = [A.alloc([512], F32) for _ in range(NQ)]
        sg_ = [A.alloc([512], F32) for _ in range(NQ)]
        ut_ = [A.alloc([512], F32) for _ in range(NQ)]
        qb4 = bufs(NQ)
        xm4 = [A.alloc([D], F32) for _ in range(2)]
        xm4b = bufs(2)
        y4 = [A.alloc([D], F32) for _ in range(2)]
        y4b = bufs(2)
        st64 = [A.alloc([12], F32) for _ in range(2)]
        mv4 = [A.alloc([2], F32) for _ in range(2)]
        rs4 = [A.alloc([1], F32) for _ in range(2)]
        stb4 = bufs(2)
        hb = Buf()
        wgb_ = Buf()
        wdb_ = Buf()
        acb = bufs(NTC)
        atb = bufs(TC // 512)
        wtb = Buf()
        qc = 0
        for ck in range(NCK):
            tk0 = ck * NTC
            S.dma('sp', hch, h2T[:, ck * TC:(ck + 1) * TC].rearrange("(kt p) n -> p kt n", p=128), W=[hb])
            S.dma('sp', wrt, wrtD[ck * TC:(ck + 1) * TC, :].rearrange("(a p) n -> p a n", p=128), W=[wrb4])
            for tt_ in range(NTC):
                pt, pb = psum()
                S.tp(pt[0:NE, 0:128], wrt[:, tt_, :], identf, R=[wrb4], W=[pb])
                S.cp('act', wrtT[0:NE, tt_, :], pt[0:NE, 0:128], R=[pb], W=[wtb])
                for hf in range(2):
                    pt2, pb2 = psum()
                    S.mm(pt2[:, :], wrtT[0:NE, tt_, :], bd_sb[0:NE, hf * 512:(hf + 1) * 512], R=[wtb, p4], W=[pb2])
                    S.cp('act', acc4[:, tt_, hf * 512:(hf + 1) * 512], pt2[:, :], R=[pb2], W=[acb[tt_]])
            for e in range(NE):
                S.dma('sp', wg_sb, wgub[e].rearrange("(kt p) n -> p kt n", p=128), W=[wgb_])
                S.dma('pool', wd_sb, wdb[e].rearrange("(kt p) n -> p kt n", p=128), W=[wdb_])
                for gi in range(TC // 512):
                    gc0 = gi * 512
                    for c in range(8):
                        pg, pgb = psum()
                        pu, pub = psum()
                        for kt in range(NKT):
                            S.mm(pg[:, :], wg_sb[:, kt, c * 128:(c + 1) * 128], hch[:, kt, gc0:gc0 + 512],
                                 start=(kt == 0), stop=(kt == NKT - 1), R=[wgb_, hb], W=[pgb])
                        for kt in range(NKT):
                            S.mm(pu[:, :], wg_sb[:, kt, DFF + c * 128:DFF + (c + 1) * 128], hch[:, kt, gc0:gc0 + 512],
                                 start=(kt == 0), stop=(kt == NKT - 1), R=[wgb_, hb], W=[pub])
                        q = qc % NQ
                        qc += 1
                        qb = qb4[q]
                        S.ts('dve', gt_[q], pg[:, :], bgu_sb[:, e, c:c + 1], 7.0, ALU.add, ALU.min, R=[pgb], W=[qb])
                        S.act(sg_[q], gt_[q], AF.Sigmoid, scale=1.702, R=[qb], W=[qb])
                        S.ts('dve', ut_[q], pu[:, :], bgu_sb[:, e, 8 + c:9 + c], 7.0, ALU.add, ALU.min, R=[pub], W=[qb])
                        S.ts('pool', ut_[q], ut_[q], -7.0, 1.0, ALU.max, ALU.add, R=[qb], W=[qb])
                        S.tt('pool', gt_[q], gt_[q], sg_[q], ALU.mult, R=[qb], W=[qb])
                        S.tt('dve', actT[:, c, gc0:gc0 + 512], ut_[q], gt_[q], ALU.mult, R=[qb], W=[atb[gi]])
                for tt_ in range(NTC):
                    gi = (tt_ * 128) // 512
                    for hf in range(2):
                        py, pyb = psum()
                        for c in range(8):
                            S.mm(py[:, :], actT[:, c, tt_ * 128:(tt_ + 1) * 128], wd_sb[:, c, hf * 512:(hf + 1) * 512],
                                 start=(c == 0), stop=(c == 7), R=[atb[gi], wdb_], W=[pyb])
                        av = acc4[:, tt_, hf * 512:(hf + 1) * 512]
                        S.stt(av, py[:, :], wrt[:, tt_, e:e + 1], av, ALU.mult, ALU.add,
                              R=[pyb, acb[tt_], wrb4], W=[acb[tt_]])
            for tt_ in range(NTC):
                t = tk0 + tt_
                seg = t // NTS
                sl = tt_ % 2
                rows = slice(t * 128, (t + 1) * 128)
                S.dma('sp', xm4[sl], xmid[rows, :], W=[xm4b[sl]])
                S.tt('pool', y4[sl], acc4[:, tt_, :], g2b[:, seg, :], ALU.mult, R=[acb[tt_]], W=[y4b[sl]])
                S.stt(y4[sl], xm4[sl], ALPHA, y4[sl], ALU.mult, ALU.add, R=[xm4b[sl], y4b[sl]], W=[y4b[sl]])
                ln_stats(y4[sl], y4b[sl], st64[sl], mv4[sl], rs4[sl], stb4[sl])
                S.ts('dve', y4[sl], y4[sl], mv4[sl][:, 0:1], rs4[sl][:, 0:1], ALU.subtract, ALU.mult,
                     R=[y4b[sl], stb4[sl]], W=[y4b[sl]])
                S.tt('pool', y4[sl], y4[sl], l2g, ALU.mult, R=[y4b[sl]], W=[y4b[sl]])
                S.tt('pool', y4[sl], y4[sl], l2b, ALU.add, R=[y4b[sl]], W=[y4b[sl]])
                S.dma('sp', x_dst[rows, :], y4[sl], R=[y4b[sl]])
        S.barrier()

    S.barrier()
    with nc.Block() as block:
        S.emit(block)
    stack.close()
    return nc


def _consts():
    i = np.arange(128)
    identf = np.eye(128, dtype=np.float32)
    onesf = np.ones((128, 128), np.float32)
    cmF = (i[:, None] <= i[None, :]).astype(np.float32)
    cmB = (i[:, None] >= i[None, :]).astype(np.float32)
    negF = np.where(i[None, :] >= i[:, None], 0.0, NEGBIG).astype(np.float32)
    negB = np.where(i[None, :] <= i[:, None], 0.0, NEGBIG).astype(np.float32)
    stF = (i[None, :] > i[:, None]).astype(np.float32)
    stB = (i[None, :] < i[:, None]).astype(np.float32)
    ik = i[:, None]
    j = i[None, :]
    relA = ik - 64 - j
    relB = ik + 64 - j

    def rt(rel, extra_mask):
        ok = (np.abs(rel) <= 64) & extra_mask
        return np.where(ok, -np.abs(rel).astype(np.float32), NEGBIG).astype(np.float32)
    allk = np.ones((128, 128), bool)
    A_I = rt(relA, allk)
    B_I = rt(relB, allk)
    A_F = rt(relA, (ik >= 64) & allk)
    B_L = rt(relB, (ik < 64) & allk)
    I_ = np.concatenate([A_I, B_I], 1)
    F_ = np.concatenate([A_F, B_I], 1)
    L_ = np.concatenate([A_I, B_L], 1)
    pair = lambda a, b: np.concatenate([a, b], 1)
    FI, II, IL, FL = pair(F_, I_), pair(I_, I_), pair(I_, L_), pair(F_, L_)
    rts = np.concatenate([FI, II, IL, FL, II, II, pair(F_, I_), pair(I_, L_)], 1).astype(np.float32)
    blk = lambda s_: (i // s_)
    bd16 = (blk(16)[:, None] == blk(16)[None, :]).astype(np.float32)
    offs = [((blk(2 * s_)[:, None] == blk(2 * s_)[None, :]) & (blk(s_)[:, None] != blk(s_)[None, :])).astype(np.float32) for s_ in (16, 32, 64)]
    bmk = np.concatenate([bd16] + offs, 1)
    sel65 = np.zeros((65, 64), np.float32)
    sel65[64, :] = 1.0
    sels = np.zeros((2, 2, 128), np.float32)
    sels[0, 0] = 1
    sels[1, 1] = 1
    return dict(k_identf=identf, k_onesf=onesf, k_cm=np.concatenate([cmF, cmB], 1),
                k_negm=np.concatenate([negF, negB], 1), k_strict=np.concatenate([stF, stB], 1),
                k_r=rts, k_bm=bmk, k_sel65=sel65, k_sels=sels.reshape(2, 256))


def _weights_map(w, NL):
    f = lambda a: np.ascontiguousarray(np.asarray(a, dtype=np.float32))
    m = {}
    m["w_ada"] = f(w["w_ada"][:NL])
    m["b_ada"] = f(w["b_ada"][:NL])
    m["w_in"] = f(w["w_in"][:NL])
    cw = np.asarray(w["conv_w"][:NL], np.float32)
    m["convw"] = f(cw.reshape(NL, 5, 12, 128).transpose(0, 3, 2, 1).reshape(NL, 128, 60))
    m["alog"] = f(np.asarray(w["a_log"][:NL]).reshape(NL, 8, 1))
    m["dtb"] = f(np.asarray(w["dt_bias"][:NL]).reshape(NL, 8, 1))
    m["gnw"] = f(np.asarray(w["gdn_norm_w"][:NL]).reshape(NL, 128, 1))
    m["w_out"] = f(w["w_out"][:NL])
    m["ln1g"] = f(w["ln1_g"][:NL])
    m["ln1b"] = f(w["ln1_b"][:NL])
    m["rw"] = f(w["router_w"][:NL])
    m["rb"] = f(w["router_b"][:NL])
    m["wgu"] = f(w["w_gate_up"][:NL])
    bg = np.asarray(w["b_gate_up"][:NL], np.float32)
    m["bgu"] = f(bg.reshape(NL, 32, 16, 128).transpose(0, 3, 1, 2).reshape(NL, 128, 512))
    m["wd"] = f(w["w_down"][:NL])
    m["bd"] = f(w["b_down"][:NL])
    m["ln2g"] = f(w["ln2_g"][:NL])
    m["ln2b"] = f(w["ln2_b"][:NL])
    return m


_NC_CACHE = {}


def run_cores(core_inputs, weights, TSEG, NL, debug=False):
    key = (TSEG, NL, debug)
    if key not in _NC_CACHE:
        _NC_CACHE[key] = build(TSEG, NL, debug)
    nc = _NC_CACHE[key]
    base = _weights_map(weights, NL)
    base.update(_consts())
    in_maps = []
    for (x, c2, cfv) in core_inputs:
        m = dict(base)
        m["x"] = np.ascontiguousarray(x, dtype=np.float32)
        m["c2"] = np.ascontiguousarray(c2, dtype=np.float32)
        m["cf"] = np.full((128, 1), cfv, np.float32)
        in_maps.append(m)
    res = run_bass_kernel_spmd(nc, in_maps, core_ids=list(range(len(in_maps))))
    return res


def kernel(x_prompt, x_sample, c_prompt, c_sample, **w):
    x_prompt = np.asarray(x_prompt, np.float32)
    x_sample = np.asarray(x_sample, np.float32)
    c_prompt = np.asarray(c_prompt, np.float32)
    c_sample = np.asarray(c_sample, np.float32)
    TSEG = 4096
    cores = []
    for b in range(4):
        cores.append((x_prompt[b], np.stack([c_prompt[b], c_prompt[b]]), 1.0))
    for pr in range(2):
        xs = np.concatenate([x_sample[2 * pr], x_sample[2 * pr + 1]], 0)
        cores.append((xs, np.stack([c_sample[2 * pr], c_sample[2 * pr + 1]]), 0.0))
    cores.append(cores[4])
    cores.append(cores[5])
    res = run_cores(cores, w, TSEG, 2)
    ys = [np.asarray(r["y"], np.float32) for r in res.results]
    y_prompt = np.stack(ys[0:4], 0)
    y_sample = np.stack([ys[4][:4096], ys[4][4096:], ys[5][:4096], ys[5][4096:]], 0)
    return (y_prompt, y_sample)
```

```python
import numpy as np
from contextlib import ExitStack
import ml_dtypes
import concourse.bass as bass
import concourse.mybir as mybir
from concourse.bass_utils import run_bass_kernel_spmd

F32 = mybir.dt.float32
BF16 = mybir.dt.bfloat16
AF = mybir.ActivationFunctionType
ALU = mybir.AluOpType
AX = mybir.AxisListType

D = 1024
NKT = 8
IN_COLS = 3600
NE = 32
DFF = 1024
ALPHA = 4.0 ** 0.25
LN_EPS = 1e-5
NORM_EPS = 1e-6
PAD = 1024
DILS = (1, 4, 16)
NEGBIG = -1.0e6
ENG = ('pe', 'act', 'dve', 'pool', 'sp')


def _bytes(dt):
    return 4 if dt == F32 else 2


class Buf:
    __slots__ = ('w', 'r', 'rd')

    def __init__(self):
        self.w = None
        self.r = {}
        self.rd = []


def bufs(n):
    return [Buf() for _ in range(n)]


class Sched:
    def __init__(self, nc, stack):
        self.nc = nc
        self.stack = stack
        self.streams = {e: [] for e in ENG}
        self.sem = {}
        self.cnt = {}
        self.nep = 0
        for e in ('pe', 'act', 'dve', 'pool'):
            self._epoch(e)
        self.seen = {e: {} for e in ENG}
        self.NDS = {'sp': 16, 'pool': 8, 'act': 4}
        self.dsem = {q: [stack.enter_context(nc.semaphore("dq%s%d" % (q, i))) for i in range(n)]
                     for q, n in self.NDS.items()}
        self.dk = {q: 0 for q in self.NDS}
        self.dtick = {q: [None] * n for q, n in self.NDS.items()}
        self.last = {}

    def _epoch(self, e):
        self.sem[e] = self.stack.enter_context(self.nc.semaphore("c%s%d" % (e, self.nep)))
        self.nep += 1
        self.cnt[e] = 0

    def op(self, eng, fn, R=(), W=(), dma=False):
        deps = []
        for b in R:
            if b.w is not None:
                deps.append(b.w)
        for b in W:
            if b.w is not None:
                deps.append(b.w)
            deps.extend(b.r.values())
            deps.extend(b.rd)
        if dma:
            nds = self.NDS[eng]
            dk = self.dk[eng]
            slot = dk % nds
            if self.dtick[eng][slot] is not None:
                deps.append(self.dtick[eng][slot])
            tick = (self.dsem[eng][slot], 16 * (dk // nds + 1), 'dma')
            self.dtick[eng][slot] = tick
            self.dk[eng] = dk + 1
        else:
            if self.cnt[eng] >= 30000:
                self._epoch(eng)
            self.cnt[eng] += 1
            tick = (self.sem[eng], self.cnt[eng], eng)
            self.last[eng] = tick
        waits = []
        seen = self.seen[eng]
        for (s, v, se) in deps:
            if se == 'pe' and eng == 'pe' and not dma:
                continue
            k = id(s)
            if seen.get(k, 0) >= v:
                continue
            seen[k] = v
            waits.append((s, v))
        self.streams[eng].append((waits, fn, tick))
        for b in R:
            if dma:
                b.rd.append(tick)
            else:
                b.r[eng] = tick
        for b in W:
            b.w = tick
            b.r = {}
            b.rd = []
        return tick

    def barrier(self):
        ticks = [t for t in self.last.values()] + [t for q in self.dtick for t in self.dtick[q] if t is not None]
        for e in ENG:
            waits = []
            seen = self.seen[e]
            for (s, v, se) in ticks:
                k = id(s)
                if seen.get(k, 0) >= v:
                    continue
                seen[k] = v
                waits.append((s, v))
            self.streams[e].append((waits, None, None))

    def emit(self, block):
        nc = self.nc

        def mk(name):
            def body(e):
                for waits, fn, tick in self.streams[name]:
                    for s, v in waits:
                        e.wait_ge(s, v)
                    if fn is not None:
                        ins = fn(e)
                        ins.then_inc(tick[0], 16 if tick[2] == 'dma' else 1)
            return body
        block.tensor(mk('pe'))
        block.scalar(mk('act'))
        block.vector(mk('dve'))
        block.gpsimd(mk('pool'))
        block.sync(mk('sp'))

    def mm(self, out, lhsT, rhs, start=True, stop=True, R=(), W=()):
        self.op('pe', lambda e: e.matmul(out, lhsT=lhsT, rhs=rhs, start=start, stop=stop), R, W)

    def tp(self, out, in_, ident, R=(), W=()):
        self.op('pe', lambda e: e.transpose(out, in_, ident), R, W)

    def act(self, out, in_, func, bias=None, scale=None, R=(), W=()):
        kw = {}
        if bias is not None:
            kw['bias'] = bias
        if scale is not None:
            kw['scale'] = scale
        self.op('act', lambda e: e.activation(out, in_, func, **kw), R, W)

    def ts(self, eng, out, in0, s1, s2, op0, op1=None, R=(), W=()):
        if op1 is None:
            self.op(eng, lambda e: e.tensor_scalar(out, in0, s1, None, op0), R, W)
        else:
            self.op(eng, lambda e: e.tensor_scalar(out, in0, s1, s2, op0, op1), R, W)

    def stt(self, out, in0, scalar, in1, op0, op1, R=(), W=(), accum=None):
        if accum is None:
            self.op('dve', lambda e: e.scalar_tensor_tensor(out, in0, scalar, in1, op0, op1), R, W)
        else:
            self.op('dve', lambda e: e.scalar_tensor_tensor(out, in0, scalar, in1, op0, op1, accum_out=accum), R, W)

    def tt(self, eng, out, in0, in1, op, R=(), W=()):
        self.op(eng, lambda e: e.tensor_tensor(out, in0, in1, op), R, W)

    def cp(self, eng, out, in_, R=(), W=()):
        if eng == 'act':
            self.op('act', lambda e: e.copy(out, in_), R, W)
        else:
            self.op(eng, lambda e: e.tensor_copy(out, in_), R, W)

    def memset(self, eng, ap, val, W=()):
        self.op(eng, lambda e: e.memset(ap, val), (), W)

    def dma(self, eng, out, in_, R=(), W=(), slow=False):
        if slow:
            self.op(eng, lambda e: e.dma_start(out=out, in_=in_, allow_slow_non_contiguous=True), R, W, dma=True)
        else:
            self.op(eng, lambda e: e.dma_start(out=out, in_=in_), R, W, dma=True)


class Arena:
    def __init__(self, ap, nbytes):
        self.ap = ap
        self.n = nbytes
        self.top = 0

    def alloc(self, shape, dt, parts=128):
        n = 1
        for s in shape:
            n *= s
        size = (n * _bytes(dt) + 63) // 64 * 64
        off = self.top
        self.top += size
        assert self.top <= self.n, "SBUF arena overflow %d > %d" % (self.top, self.n)
        v = self.ap[0:parts, off // 2:(off + n * _bytes(dt)) // 2]
        if dt != BF16:
            v = v.bitcast(dt)
        if len(shape) == 2:
            v = v.rearrange("p (a b) -> p a b", a=shape[0])
        elif len(shape) == 3:
            v = v.rearrange("p (a b c) -> p a b c", a=shape[0], b=shape[1])
        return v


def run_layers(rounds, interleave=True):
    gens = list(rounds)
    while gens:
        nxt = []
        for g in gens:
            try:
                next(g)
                nxt.append(g)
            except StopIteration:
                pass
        gens = nxt


def build(TSEG, NL, debug=False):
    T = 2 * TSEG
    NT = T // 128
    NTS = TSEG // 128
    NG = T // 512
    nc = bass.Bass("TRN2", target_bir_lowering=False)
    stack = ExitStack()

    def din(name, shape, dt=F32):
        return nc.dram_tensor(name, list(shape), dt, kind="ExternalInput").ap()

    skind = "ExternalOutput" if debug else "Internal"

    def dscr(name, shape, dt):
        return nc.dram_tensor(name, list(shape), dt, kind=skind).ap()

    x_in = din("x", [T, D])
    c2_in = din("c2", [2, D])
    cf_in = din("cf", [128, 1])
    w_ada = din("w_ada", [NL, D, 6 * D])
    b_ada = din("b_ada", [NL, 6 * D])
    w_in = din("w_in", [NL, D, IN_COLS])
    convw = din("convw", [NL, 128, 60])
    alog = din("alog", [NL, 8, 1])
    dtb = din("dtb", [NL, 8, 1])
    gnw = din("gnw", [NL, 128, 1])
    w_out = din("w_out", [NL, D, D])
    ln1g = din("ln1g", [NL, D])
    ln1b = din("ln1b", [NL, D])
    rw = din("rw", [NL, D, NE])
    rb = din("rb", [NL, NE])
    wgu = din("wgu", [NL, NE, D, 2 * DFF])
    bgu = din("bgu", [NL, 128, NE * 16])
    wd = din("wd", [NL, NE, DFF, D])
    bd = din("bd", [NL, NE, D])
    ln2g = din("ln2g", [NL, D])
    ln2b = din("ln2b", [NL, D])
    k_identf = din("k_identf", [128, 128])
    k_onesf = din("k_onesf", [128, 128])
    k_cm = din("k_cm", [128, 2 * 128])
    k_negm = din("k_negm", [128, 2 * 128])
    k_strict = din("k_strict", [128, 2 * 128])
    k_bm = din("k_bm", [128, 4 * 128])
    k_r = din("k_r", [128, 8 * 512])
    k_sel65 = din("k_sel65", [65, 64])
    k_sels = din("k_sels", [2, 256])
    y_out = nc.dram_tensor("y", [T, D], F32, kind="ExternalOutput").ap()

    winb = dscr("winb", [D, IN_COLS], BF16)
    woutb = dscr("woutb", [D, D], BF16)
    wgub = dscr("wgub", [NE, D, 2 * DFF], BF16)
    wdb = dscr("wdb", [NE, DFF, D], BF16)
    modrows = dscr("modrows", [2, 6 * D], F32)
    gdnT = dscr("gdnT", [2048, T], F32)
    abT = dscr("abT", [16, T], F32)
    attT = dscr("attT", [1536, T], BF16)
    mixT = dscr("mixT", [D, T], BF16)
    xmid = dscr("xmid", [T, D], F32)
    h2T = dscr("h2T", [D, T], BF16)
    x1 = dscr("x1", [T, D], F32)
    wrtD = dscr("wrtD", [T, NE], F32)

    ARENA_B = 200 * 1024
    arena_t = stack.enter_context(nc.sbuf_tensor("arena", [128, ARENA_B // 2], BF16))
    ps_t = [stack.enter_context(nc.psum_tensor("ps%d" % i, [128, 512], F32)) for i in range(8)]
    S = Sched(nc, stack)
    A = Arena(arena_t, ARENA_B)
    psb = bufs(8)
    pctr = [0]

    def psum():
        i = pctr[0] % 8
        pctr[0] += 1
        return ps_t[i], psb[i]

    identf = A.alloc([128], F32)
    identb = A.alloc([128], BF16)
    onesf = A.alloc([128], F32)
    onesb = A.alloc([128], BF16)
    cm = A.alloc([2, 128], F32)
    negm = A.alloc([2, 128], F32)
    strict = A.alloc([2, 128], F32)
    bm = A.alloc([4, 128], F32)
    sel65 = A.alloc([64], F32)
    cf = A.alloc([1], F32)
    epsln = A.alloc([1], F32)
    epsnm = A.alloc([1], F32)
    cb = Buf()
    S.dma('sp', identf, k_identf, W=[cb])
    S.dma('sp', onesf, k_onesf, W=[cb])
    S.dma('sp', cm, k_cm.rearrange("p (a b) -> p a b", a=2), W=[cb])
    S.dma('sp', negm, k_negm.rearrange("p (a b) -> p a b", a=2), W=[cb])
    S.dma('sp', strict, k_strict.rearrange("p (a b) -> p a b", a=2), W=[cb])
    S.dma('sp', bm, k_bm.rearrange("p (a b) -> p a b", a=4), W=[cb])
    S.dma('sp', sel65[0:65], k_sel65, W=[cb])
    S.dma('sp', cf, cf_in, W=[cb])
    S.barrier()
    S.cp('dve', identb, identf)
    S.cp('dve', onesb, onesf)
    S.memset('dve', epsln, LN_EPS)
    S.memset('dve', epsnm, NORM_EPS)
    S.barrier()
    persist_top = A.top

    def ln_stats(xt, xb, st6, mv, rstd, tb):
        S.op('dve', lambda e: e.bn_stats(st6[:, 0:6], xt[:, 0:512]), [xb], [tb])
        S.op('dve', lambda e: e.bn_stats(st6[:, 6:12], xt[:, 512:1024]), [xb], [tb])
        S.op('dve', lambda e: e.bn_aggr(mv, st6), [tb], [tb])
        S.act(rstd, mv[:, 1:2], AF.Sqrt, bias=epsln[:, 0:1], scale=1.0, R=[tb], W=[tb])
        S.op('dve', lambda e: e.reciprocal(rstd, rstd), [tb], [tb])

    for L in range(NL):
        x_src = x_in if L == 0 else x1
        x_dst = y_out if L == NL - 1 else x1
        A.top = persist_top
        NSL = 3
        stf = [A.alloc([2, 2048], F32) for _ in range(NSL)]
        stb = [A.alloc([2, 2048], BF16) for _ in range(NSL)]
        sfb = bufs(NSL)
        sbb = bufs(NSL)
        jobs = []
        for e in range(NE):
            for q in range(4):
                jobs.append((wgu[L, e, q * 256:(q + 1) * 256, :].rearrange("(a p) n -> p a n", p=128),
                             wgub[e, q * 256:(q + 1) * 256, :].rearrange("(a p) n -> p a n", p=128), (2, 2048)))
            for q in range(2):
                jobs.append((wd[L, e, q * 512:(q + 1) * 512, :].rearrange("(a p) n -> p a n", p=128),
                             wdb[e, q * 512:(q + 1) * 512, :].rearrange("(a p) n -> p a n", p=128), (4, 1024)))
        for q in range(8):
            jobs.append((w_in[L, q * 128:(q + 1) * 128, :].rearrange("(a p) n -> p a n", p=128),
                         winb[q * 128:(q + 1) * 128, :].rearrange("(a p) n -> p a n", p=128), (1, 3600)))
        for q in range(2):
            jobs.append((w_out[L, q * 512:(q + 1) * 512, :].rearrange("(a p) n -> p a n", p=128),
                         woutb[q * 512:(q + 1) * 512, :].rearrange("(a p) n -> p a n", p=128), (4, 1024)))
        ceng = ('act', 'dve', 'pool')
        for j, (src, dst, (a, n)) in enumerate(jobs):
            sl = j % NSL
            fv = stf[sl].rearrange("p a b -> p (a b)")[:, 0:a * n].rearrange("p (a b) -> p a b", a=a)
            bv = stb[sl].rearrange("p a b -> p (a b)")[:, 0:a * n].rearrange("p (a b) -> p a b", a=a)
            S.dma('sp', fv, src, W=[sfb[sl]])
            S.cp(ceng[j % 3], bv, fv, R=[sfb[sl]], W=[sbb[sl]])
            S.dma('pool' if j % 2 else 'sp', dst, bv, R=[sbb[sl]])
        S.barrier()

        A.top = persist_top
        cT = A.alloc([NKT, 2], F32)
        cTs = A.alloc([NKT, 2], F32)
        wa = [A.alloc([NKT, 512], F32) for _ in range(2)]
        wab = bufs(2)
        brow = A.alloc([6 * D], F32)
        mrow = A.alloc([6 * D], F32)
        b0 = Buf()
        for s in range(2):
            for kt in range(NKT):
                S.dma('sp', cT[:, kt, s:s + 1], c2_in[s:s + 1, kt * 128:(kt + 1) * 128].rearrange("s p -> p s"),
                      W=[b0], slow=True)
        S.dma('sp', brow[0:2], b_ada[L:L + 1, :].partition_broadcast(2).rearrange("p a n -> p (a n)"), W=[b0])
        S.act(cTs, cT, AF.Silu, R=[b0], W=[b0])
        for j in range(12):
            sl = j % 2
            S.dma('sp', wa[sl], w_ada[L, :, j * 512:(j + 1) * 512].rearrange("(kt p) n -> p kt n", p=128), W=[wab[sl]])
            pt, pb = psum()
            for kt in range(NKT):
                S.mm(pt[0:2, :], cTs[:, kt, :], wa[sl][:, kt, :], start=(kt == 0), stop=(kt == NKT - 1),
                     R=[wab[sl], b0], W=[pb])
            S.tt('dve', mrow[0:2, j * 512:(j + 1) * 512], pt[0:2, :], brow[0:2, j * 512:(j + 1) * 512], ALU.add,
                 R=[pb, b0], W=[b0])
        S.dma('sp', modrows, mrow[0:2], R=[b0])
        S.barrier()

        def load_mod_cols(dst, blk, tb):
            for s in range(2):
                S.dma('sp', dst[:, s, :], modrows[s, blk * D:(blk + 1) * D].rearrange("(kt p) -> p kt", p=128),
                      W=[tb], slow=True)

        def bcast_row(dst, row_ap, tb, eng='sp'):
            S.dma(eng, dst, row_ap.partition_broadcast(128).rearrange("p a n -> p (a n)"), W=[tb])

        A.top = persist_top
        wsb = A.alloc([NKT, IN_COLS], BF16)
        sh1 = A.alloc([2, 8], F32)
        sc1 = A.alloc([2, 8], F32)
        p1b = Buf()
        S.dma('sp', wsb, winb.rearrange("(kt p) n -> p kt n", p=128), W=[p1b])
        load_mod_cols(sh1, 0, p1b)
        load_mod_cols(sc1, 1, p1b)
        S.ts('dve', sc1, sc1, 1.0, None, ALU.add, R=[p1b], W=[p1b])
        S.barrier()
        NX = 3
        xt_ = [A.alloc([D], F32) for _ in range(NX)]
        xtb = bufs(NX)
        xn_ = [A.alloc([D], BF16) for _ in range(NX)]
        xnb = bufs(NX)
        st6 = [A.alloc([12], F32) for _ in range(NX)]
        mv_ = [A.alloc([2], F32) for _ in range(NX)]
        rs_ = [A.alloc([1], F32) for _ in range(NX)]
        stb_ = bufs(NX)
        hT = [A.alloc([NKT, 512], BF16) for _ in range(2)]
        hTb = bufs(2)
        NO = 4
        ostf = [A.alloc([512], F32) for _ in range(NO)]
        ostb = [A.alloc([512], BF16) for _ in range(NO)]
        ofb = bufs(NO)
        obb = bufs(NO)
        colchunks = [(i * 128, 128) for i in range(16)] + [(2048, 16)] + [(2064 + i * 128, 128) for i in range(12)]
        tcount = 0
        ocount = 0
        for g in range(NG):
            hs = g % 2
            for ti in range(4):
                t = g * 4 + ti
                seg = t // NTS
                sl = tcount % NX
                tcount += 1
                S.dma('sp', xt_[sl], x_src[t * 128:(t + 1) * 128, :], W=[xtb[sl]])
                ln_stats(xt_[sl], xtb[sl], st6[sl], mv_[sl], rs_[sl], stb_[sl])
                S.ts('dve', xn_[sl], xt_[sl], mv_[sl][:, 0:1], rs_[sl][:, 0:1], ALU.subtract, ALU.mult,
                     R=[xtb[sl], stb_[sl]], W=[xnb[sl]])
                pt, pb = psum()
                ptb = pt[:, :].bitcast(BF16).rearrange("p (a b) -> p a b", a=8)
                for kt in range(NKT):
                    S.tp(ptb[:, kt, :], xn_[sl][:, kt * 128:(kt + 1) * 128], identb, R=[xnb[sl]], W=[pb])
                for kt in range(NKT):
                    S.act(hT[hs][:, kt, ti * 128:(ti + 1) * 128], ptb[:, kt, :], AF.Identity,
                          bias=sh1[:, seg, kt:kt + 1], scale=sc1[:, seg, kt:kt + 1], R=[pb], W=[hTb[hs]])
            for ci, (c0, cn) in enumerate(colchunks):
                pt, pb = psum()
                for kt in range(NKT):
                    S.mm(pt[0:cn, :], wsb[:, kt, c0:c0 + cn], hT[hs][:, kt, :], start=(kt == 0), stop=(kt == NKT - 1),
                         R=[hTb[hs]], W=[pb])
                sl = ocount % NO
                ocount += 1
                ev = 'act' if ci % 2 else 'dve'
                cols = slice(g * 512, (g + 1) * 512)
                if ci < 16:
                    S.cp(ev, ostf[sl][0:cn], pt[0:cn, :], R=[pb], W=[ofb[sl]])
                    S.dma('pool' if ci % 2 else 'sp', gdnT[c0:c0 + cn, cols], ostf[sl][0:cn], R=[ofb[sl]])
                elif ci == 16:
                    S.cp(ev, ostf[sl][0:cn], pt[0:cn, :], R=[pb], W=[ofb[sl]])
                    S.dma('sp', abT[:, cols], ostf[sl][0:cn], R=[ofb[sl]])
                else:
                    r0 = (ci - 17) * 128
                    if ci < 21:
                        S.ts('dve', ostb[sl], pt[:, :], 0.125, None, ALU.mult, R=[pb], W=[obb[sl]])
                    else:
                        S.cp(ev, ostb[sl], pt[:, :], R=[pb], W=[obb[sl]])
                    S.dma('pool' if ci % 2 else 'sp', attT[r0:r0 + 128, cols], ostb[sl], R=[obb[sl]])
        S.barrier()

        A.top = persist_top
        qT = A.alloc([T], BF16)
        kp = A.alloc([T + 2 * PAD], BF16)
        vp = A.alloc([T + 2 * PAD], BF16)
        NVT = 80 if T >= 2048 else 80
        vaug = A.alloc([NVT, 2, 65], BF16)
        accT = A.alloc([2, T], F32)
        tmpf = [A.alloc([512], F32) for _ in range(2)]
        tmpfb = bufs(2)
        ptb_ = [A.alloc([512], BF16) for _ in range(2)]
        ptbb = bufs(2)
        recf = [A.alloc([512], F32) for _ in range(2)]
        recb = bufs(2)
        outb = [A.alloc([512], BF16) for _ in range(2)]
        outbb = bufs(2)
        ib = Buf()
        vab = bufs(NVT)
        rt = A.alloc([8, 512], F32)
        tmpb = A.alloc([512], F32)
        S.dma('sp', rt, k_r.rearrange("p (a b) -> p a b", a=8), W=[ib])
        alts = (2, 0, 3, 3)
        for i in range(4):
            S.tt('dve', tmpb, rt[:, 4 + i, :], rt[:, alts[i], :], ALU.subtract, R=[ib], W=[ib])
            S.stt(rt[:, 4 + i, :], tmpb, cf[:, 0:1], rt[:, alts[i], :], ALU.mult, ALU.add, R=[ib], W=[ib])
        S.memset('pool', vaug, 1.0, W=[ib])
        S.memset('pool', kp[:, 0:PAD], 0.0, W=[ib])
        S.memset('pool', kp[:, PAD + T:], 0.0, W=[ib])
        S.memset('pool', vp[:, 0:PAD], 0.0, W=[ib])
        S.memset('pool', vp[:, PAD + T:], 0.0, W=[ib])
        S.barrier()
        for hp in range(4):
            lb = Buf()
            S.dma('sp', qT, attT[hp * 128:(hp + 1) * 128, :], W=[lb])
            S.dma('sp', kp[:, PAD:PAD + T], attT[512 + hp * 128:512 + (hp + 1) * 128, :], W=[lb])
            S.dma('sp', vp[:, PAD:PAD + T], attT[1024 + hp * 128:1024 + (hp + 1) * 128, :], W=[lb])
            S.barrier()
            accb = [bufs(NG) for _ in range(2)]
            cnt = 0
            for pi, dil in enumerate(DILS):
                U = T // dil
                US = TSEG // dil
                nq = U // 128
                nqs = US // 128
                nkt = nq + 1
                vb = vab
                vi = 0
                for r in range(dil):
                    for k0 in range(0, nkt, 8):
                        kn = min(8, nkt - k0)
                        pt, pb = psum()
                        pv = pt[:, :].bitcast(BF16).rearrange("p (a b) -> p a b", a=8)
                        for kk in range(kn):
                            kt = k0 + kk
                            st = PAD + r + dil * (128 * kt - 64)
                            S.tp(pv[:, kk, :], vp[:, st:st + dil * 127 + 1:dil], identb, W=[pb])
                        sl0 = r * nkt + k0
                        S.cp('act' if vi % 2 else 'dve',
                             vaug[:, sl0:sl0 + kn, :, 0:64],
                             pv[:, 0:kn, :].rearrange("p a (h d) -> p a h d", h=2),
                             R=[pb], W=vb[sl0:sl0 + kn])
                        vi += 1
                for h in range(2):
                    h0 = h * 64
                    chp = dil * (2.0 ** (-(2 * hp + h + 1)))
                    for r in range(dil):
                        for qp in range(0, nq, 4):
                            po, pob = psum()
                            for q2 in (0, 2):
                                qa = qp + q2
                                segq = qa // nqs
                                la = qa % nqs
                                first = (la == 0)
                                lastp = (la + 2 == nqs)
                                if nqs == 2:
                                    ridx = 6 if segq == 0 else 7
                                    if segq == 0:
                                        ridx = 6
                                    else:
                                        ridx = 7
                                elif first:
                                    ridx = 0 if segq == 0 else 5
                                elif lastp:
                                    ridx = 4 if segq == 0 else 2
                                else:
                                    ridx = 1
                                pt, pb = psum()
                                for qq in range(2):
                                    q = qa + qq
                                    qs = r + dil * 128 * q
                                    for half in range(2):
                                        ks = PAD + r + dil * (128 * (q + half) - 64)
                                        S.mm(pt[:, qq * 256 + half * 128: qq * 256 + (half + 1) * 128],
                                             kp[h0:h0 + 64, ks:ks + dil * 127 + 1:dil],
                                             qT[h0:h0 + 64, qs:qs + dil * 127 + 1:dil], W=[pb])
                                sl = cnt % 2
                                cnt += 1
                                S.stt(tmpf[sl], rt[:, ridx, :], chp, pt[:, :], ALU.mult, ALU.add,
                                      R=[pb], W=[tmpfb[sl]])
                                S.act(ptb_[sl], tmpf[sl], AF.Exp, R=[tmpfb[sl]], W=[ptbb[sl]])
                                for qq in range(2):
                                    q = qa + qq
                                    for half in range(2):
                                        vt = r * nkt + q + half
                                        S.mm(po[0:65, (q2 + qq) * 128:(q2 + qq + 1) * 128],
                                             vaug[:, vt, h, :],
                                             ptb_[sl][:, qq * 256 + half * 128: qq * 256 + (half + 1) * 128],
                                             start=(half == 0), stop=(half == 1),
                                             R=[ptbb[sl], vb[vt]], W=[pob])
                            ts0 = r + dil * 128 * qp
                            dst = accT[0:65, h, ts0:ts0 + dil * 511 + 1:dil]
                            glo = ts0 // 512
                            ghi = (ts0 + dil * 512 - 1) // 512
                            ab_ = accb[h][glo:ghi + 1]
                            if pi == 0:
                                S.cp('act', dst, po[0:65, :], R=[pob], W=ab_)
                            else:
                                S.tt('dve', dst, po[0:65, :], dst, ALU.add, R=[pob] + ab_, W=ab_)
            for h in range(2):
                for g in range(NG):
                    cols = slice(g * 512, (g + 1) * 512)
                    pt, pb = psum()
                    S.mm(pt[0:64, :], sel65[0:65, :], accT[0:65, h, cols], R=[accb[h][g]], W=[pb])
                    sl = g % 2
                    S.op('dve', lambda e, o=recf[sl][0:64], i=pt[0:64, :]: e.reciprocal(o, i), [pb], [recb[sl]])
                    S.tt('pool', outb[sl][0:64], accT[0:64, h, cols], recf[sl][0:64], ALU.mult,
                         R=[recb[sl], accb[h][g]], W=[outbb[sl]])
                    r0 = 512 + (2 * hp + h) * 64
                    S.dma('sp', mixT[r0:r0 + 64, cols], outb[sl][0:64], R=[outbb[sl]])
            S.barrier()

        A.top = persist_top
        gtok = A.alloc([NT, 8], F32)
        btok = A.alloc([NT, 8], F32)
        gcF = A.alloc([NT, 8], F32)
        gcB = A.alloc([NT, 8], F32)
        gtot = A.alloc([NT, 8], F32)
        negegcF = A.alloc([NT, 8], F32)
        negegcB = A.alloc([NT, 8], F32)
        kdecF = A.alloc([NT, 8], F32)
        kdecB = A.alloc([NT, 8], F32)
        glast = A.alloc([NT, 8], F32)
        nbeta = A.alloc([NT, 8], F32)
        dt8 = A.alloc([1], F32)
        na8 = A.alloc([1], F32)
        cw = A.alloc([60], F32)
        gw = A.alloc([1], F32)
        gdn_top = A.top
        ga = A.alloc([T], F32)
        gbta = A.alloc([T], F32)
        pb0 = Buf()
        S.dma('sp', ga[0:8], abT[0:8, :], W=[pb0])
        S.dma('sp', gbta[0:8], abT[8:16, :], W=[pb0])
        S.dma('sp', dt8[0:8], dtb[L], W=[pb0])
        S.dma('sp', na8[0:8], alog[L], W=[pb0])
        S.dma('sp', cw, convw[L], W=[pb0])
        S.dma('sp', gw, gnw[L], W=[pb0])
        S.act(na8[0:8], na8[0:8], AF.Exp, R=[pb0], W=[pb0])
        S.ts('dve', na8[0:8], na8[0:8], -1.0, None, ALU.mult, R=[pb0], W=[pb0])
        S.act(ga[0:8], ga[0:8], AF.Exp, bias=dt8[0:8, 0:1], scale=1.0, R=[pb0], W=[pb0])
        S.ts('dve', ga[0:8], ga[0:8], 1.0, None, ALU.add, R=[pb0], W=[pb0])
        S.act(ga[0:8], ga[0:8], AF.Ln, R=[pb0], W=[pb0])
        S.ts('dve', ga[0:8], ga[0:8], na8[0:8, 0:1], None, ALU.mult, R=[pb0], W=[pb0])
        S.act(gbta[0:8], gbta[0:8], AF.Sigmoid, R=[pb0], W=[pb0])
        for (srcr, dsttok) in ((ga, gtok), (gbta, btok)):
            for t0 in range(0, NT, 64):
                tn = min(64, NT - t0)
                pt, pb = psum()
                pv = pt[:, :].rearrange("p (a b) -> p a b", b=8)
                for tt_ in range(tn):
                    t = t0 + tt_
                    S.tp(pv[:, tt_, :], srcr[0:8, t * 128:(t + 1) * 128], identf[0:8, 0:8], R=[pb0], W=[pb])
                S.cp('dve', dsttok[:, t0:t0 + tn, :], pv[:, 0:tn, :], R=[pb], W=[pb0])
        for t0 in range(0, NT, 64):
            tn = min(64, NT - t0)
            rhs = gtok[:, t0:t0 + tn, :].rearrange("p a b -> p (a b)")
            for (dstt, lhs) in ((gcF, cm[:, 0, :]), (gcB, cm[:, 1, :]), (gtot, onesf)):
                pt, pb = psum()
                S.mm(pt[:, 0:tn * 8], lhs, rhs, R=[pb0], W=[pb])
                S.cp('dve', dstt[:, t0:t0 + tn, :].rearrange("p a b -> p (a b)"), pt[:, 0:tn * 8], R=[pb], W=[pb0])
        for (gc_, ne_, kd_) in ((gcF, negegcF, kdecF), (gcB, negegcB, kdecB)):
            S.act(ne_, gc_, AF.Exp, R=[pb0], W=[pb0])
            S.ts('dve', ne_, ne_, -1.0, None, ALU.mult, R=[pb0], W=[pb0])
            S.tt('dve', kd_, gtot, gc_, ALU.subtract, R=[pb0], W=[pb0])
            S.act(kd_, kd_, AF.Exp, R=[pb0], W=[pb0])
        S.act(glast, gtot, AF.Exp, R=[pb0], W=[pb0])
        S.ts('dve', nbeta, btok, -1.0, None, ALU.mult, R=[pb0], W=[pb0])
        S.barrier()
        for hd in range(4):
            A.top = gdn_top
            qTg = A.alloc([T], BF16)
            kTg = A.alloc([T], BF16)
            ktok = A.alloc([NT, 128], BF16)
            vtok = A.alloc([NT, 128], BF16)
            oacc = A.alloc([NT, 128], F32)
            head_top = A.top
            CH = 1024 if T >= 1024 else T
            NCH = T // CH
            xr = [A.alloc([CH + 4], F32) for _ in range(2)]
            xrb = bufs(2)
            cacc = [A.alloc([CH], F32) for _ in range(2)]
            caccb = bufs(2)
            ysl = [A.alloc([CH], F32) for _ in range(2)]
            yslb = bufs(2)
            sq = A.alloc([CH], F32)
            sqb = Buf()
            rn = A.alloc([CH], F32)
            rnb = Buf()
            vTb = A.alloc([CH], BF16)
            vTbb = Buf()
            chb = bufs(NCH)
            jc = 0
            for which in range(3):
                row0 = which * 512 + hd * 128
                for c in range(NCH):
                    t0 = c * CH
                    sl = jc % 2
                    jc += 1
                    lo = t0 - 2
                    hi = t0 + CH + 2
                    X = xr[sl]
                    if lo < 0:
                        S.memset('pool', X[:, 0:2], 0.0, W=[xrb[sl]])
                        S.dma('sp', X[:, 2:CH + 2], gdnT[row0:row0 + 128, t0:t0 + CH], W=[xrb[sl]])
                        if hi > T:
                            S.memset('pool', X[:, CH + 2:CH + 4], 0.0, W=[xrb[sl]])
                        else:
                            S.dma('sp', X[:, CH + 2:CH + 4], gdnT[row0:row0 + 128, t0 + CH:t0 + CH + 2], W=[xrb[sl]])
                    elif hi > T:
                        S.dma('sp', X[:, 0:CH + 2], gdnT[row0:row0 + 128, lo:t0 + CH], W=[xrb[sl]])
                        S.memset('pool', X[:, CH + 2:CH + 4], 0.0, W=[xrb[sl]])
                    else:
                        S.dma('sp', X[:, :], gdnT[row0:row0 + 128, lo:hi], W=[xrb[sl]])
                    if t0 == TSEG:
                        S.ts('pool', X[:, 0:2], X[:, 0:2], cf[:, 0:1], None, ALU.mult, R=[xrb[sl]], W=[xrb[sl]])
                    if t0 + CH == TSEG:
                        S.ts('pool', X[:, CH + 2:CH + 4], X[:, CH + 2:CH + 4], cf[:, 0:1], None, ALU.mult,
                             R=[xrb[sl]], W=[xrb[sl]])
                    wbase = (which * 4 + hd) * 5
                    S.ts('dve', cacc[sl], X[:, 0:CH], cw[:, wbase:wbase + 1], None, ALU.mult,
                         R=[xrb[sl], pb0], W=[caccb[sl]])
                    for j in range(1, 5):
                        S.stt(cacc[sl], X[:, j:j + CH], cw[:, wbase + j:wbase + j + 1], cacc[sl], ALU.mult, ALU.add,
                              R=[xrb[sl], caccb[sl]], W=[caccb[sl]])
                    S.act(ysl[sl], cacc[sl], AF.Silu, R=[caccb[sl]], W=[yslb[sl]])
                    if which < 2:
                        S.act(sq, ysl[sl], AF.Square, R=[yslb[sl]], W=[sqb])
                        for hf in range(CH // 512):
                            pt, pb = psum()
                            S.mm(pt[:, :], onesf, sq[:, hf * 512:(hf + 1) * 512], R=[sqb], W=[pb])
                            S.act(rn[:, hf * 512:(hf + 1) * 512], pt[:, :], AF.Sqrt, bias=epsnm[:, 0:1], scale=1.0,
                                  R=[pb], W=[rnb])
                        S.op('dve', lambda e, o=rn: e.reciprocal(o, o), [rnb], [rnb])
                        dstT = qTg if which == 0 else kTg
                        S.stt(dstT[:, t0:t0 + CH], ysl[sl], (128.0 ** -0.5) if which == 0 else 1.0, rn,
                              ALU.mult, ALU.mult, R=[yslb[sl], rnb], W=[chb[c]])
                        srcT = dstT
                        sb_ = chb[c]
                    else:
                        S.cp('pool', vTb, ysl[sl], R=[yslb[sl]], W=[vTbb])
                        srcT = None
                    if which >= 1:
                        dtok = ktok if which == 1 else vtok
                        for k0 in range(0, CH // 128, 8):
                            pt, pb = psum()
                            pv = pt[:, :].bitcast(BF16).rearrange("p (a b) -> p a b", a=8)
                            for kk in range(8):
                                cc0 = (k0 + kk) * 128
                                if which == 1:
                                    S.tp(pv[:, kk, :], kTg[:, t0 + cc0:t0 + cc0 + 128], identb, R=[chb[c]], W=[pb])
                                else:
                                    S.tp(pv[:, kk, :], vTb[:, cc0:cc0 + 128], identb, R=[vTbb], W=[pb])
                            tl0 = t0 // 128 + k0
                            S.cp('act', dtok[:, tl0:tl0 + 8, :], pv, R=[pb], W=[chb[c]])
            S.barrier()
            A.top = head_top
            RING = 4
            TTr = [[A.alloc([128], BF16) for _ in range(RING)] for _ in range(2)]
            ITr = [[A.alloc([128], BF16) for _ in range(RING)] for _ in range(2)]
            QDr = [[A.alloc([128], BF16) for _ in range(RING)] for _ in range(2)]
            KDr = [[A.alloc([128], BF16) for _ in range(RING)] for _ in range(2)]
            rb_ = [[Buf() for _ in range(RING)] for _ in range(2)]
            dgt = [A.alloc([128], F32) for _ in range(4)]
            Et = [A.alloc([128], F32) for _ in range(4)]
            DTt = [A.alloc([128], F32) for _ in range(4)]
            DTs = [A.alloc([128], F32) for _ in range(4)]
            EGB = [A.alloc([128], BF16) for _ in range(4)]
            Pm = [A.alloc([2, 128], F32) for _ in range(4)]
            Nd = [A.alloc([2, 128], F32) for _ in range(4)]
            aD = [A.alloc([2, 128], F32) for _ in range(4)]
            No = [A.alloc([2, 128], F32) for _ in range(4)]
            Ww = [A.alloc([2, 128], F32) for _ in range(4)]
            wkb = [Buf() for _ in range(4)]
            Sf = [A.alloc([128], F32) for _ in range(2)]
            Sb = [A.alloc([128], BF16) for _ in range(2)]
            Sbuf = bufs(2)
            Xt = [A.alloc([128], BF16) for _ in range(2)]
            vnt = [A.alloc([128], BF16) for _ in range(2)]
            xb_ = bufs(2)
            ob = bufs(NT)
            visited = [False] * NT
            for d_ in range(2):
                S.memset('pool', Sf[d_], 0.0, W=[Sbuf[d_]])
                S.memset('pool', Sb[d_], 0.0, W=[Sbuf[d_]])
            order = [list(range(NT)), list(range(NT - 1, -1, -1))]

            def pre(d_, si):
                t = order[d_][si]
                w_ = 2 * d_ + (si % 2)
                ci = d_ * 4 + hd
                gc_ = (gcF, gcB)[d_]
                kd_ = (kdecF, kdecB)[d_]
                slot = si % RING
                rbuf = rb_[d_][slot]
                wb = wkb[w_]
                tc = slice(t * 128, (t + 1) * 128)
                gcol = gc_[:, t, ci:ci + 1]
                S.ts('pool', dgt[w_], identf, gcol, None, ALU.mult, W=[wb])
                pA, pAb = ps_t[4 + w_], psb[4 + w_]
                S.mm(pA[:, 0:128], onesf, dgt[w_], R=[wb], W=[pAb])
                S.mm(pA[:, 128:256], kTg[:, tc], kTg[:, tc], W=[pAb])
                S.mm(pA[:, 256:384], kTg[:, tc], qTg[:, tc], W=[pAb])
                yield
                S.stt(Et[w_], pA[:, 0:128], gcol, negm[:, d_, :], ALU.subtract, ALU.add, R=[pAb], W=[wb])
                S.act(EGB[w_], pA[:, 0:128], AF.Exp, R=[pAb], W=[wb])
                S.act(DTt[w_], Et[w_], AF.Exp, R=[wb], W=[wb])
                S.tt('pool', QDr[d_][slot], qTg[:, tc], EGB[w_], ALU.mult, R=[wb], W=[rbuf])
                S.ts('pool', KDr[d_][slot], ktok[:, t, :], kd_[:, t, ci:ci + 1], None, ALU.mult, W=[rbuf])
                S.tt('pool', DTs[w_], DTt[w_], strict[:, d_, :], ALU.mult, R=[wb], W=[wb])
                yield
                S.stt(Pm[w_][:, 0, :], pA[:, 128:256], nbeta[:, t, ci:ci + 1], DTs[w_], ALU.mult, ALU.mult,
                      R=[pAb, wb], W=[wb])
                S.tt('dve', ITr[d_][slot], pA[:, 256:384], DTt[w_], ALU.mult, R=[pAb, wb], W=[rbuf])
                pB, pBb = ps_t[4 + w_], psb[4 + w_]
                pBv = pB
                S.tp(pBv[:, 0:128], Pm[w_][:, 0, :], identf, R=[wb], W=[pBb])
                yield
                S.cp('act', Pm[w_][:, 1, :], pBv[:, 0:128], R=[pBb], W=[wb])
                yield
                PB = ps_t[4 + w_]
                PBb = psb[4 + w_]
                nd = Nd[w_]
                ad = aD[w_]
                S.tt('pool', nd[:, 0, :], Pm[w_][:, 0, :], bm[:, 0, :], ALU.mult, R=[wb], W=[wb])
                S.tt('pool', nd[:, 1, :], Pm[w_][:, 1, :], bm[:, 0, :], ALU.mult, R=[wb], W=[wb])
                S.tt('pool', ad[:, 0, :], nd[:, 0, :], identf, ALU.add, R=[wb], W=[wb])
                S.tt('pool', ad[:, 1, :], nd[:, 1, :], identf, ALU.add, R=[wb], W=[wb])
                yield
                for lvl in range(3):
                    S.mm(PB[:, 0:128], nd[:, 1, :], nd[:, 0, :], R=[wb], W=[PBb])
                    S.mm(PB[:, 128:256], nd[:, 0, :], nd[:, 1, :], R=[wb], W=[PBb])
                    yield
                    S.cp('act', nd.rearrange("p a b -> p (a b)"), PB[:, 0:256], R=[PBb], W=[wb])
                    S.mm(PB[:, 256:384], nd[:, 1, :], ad[:, 0, :], R=[wb], W=[PBb])
                    S.mm(PB[:, 384:512], nd[:, 0, :], ad[:, 1, :], R=[wb], W=[PBb])
                    yield
                    adf = ad.rearrange("p a b -> p (a b)")
                    S.tt('dve', adf, PB[:, 256:512], adf, ALU.add, R=[PBb, wb], W=[wb])
                    yield
                no = No[w_]
                ww = Ww[w_]
                for mi in (1, 2, 3):
                    S.tt('pool', no[:, 0, :], Pm[w_][:, 0, :], bm[:, mi, :], ALU.mult, R=[wb], W=[wb])
                    S.tt('pool', no[:, 1, :], Pm[w_][:, 1, :], bm[:, mi, :], ALU.mult, R=[wb], W=[wb])
                    S.mm(PB[:, 0:128], no[:, 1, :], ad[:, 0, :], R=[wb], W=[PBb])
                    S.mm(PB[:, 128:256], no[:, 0, :], ad[:, 1, :], R=[wb], W=[PBb])
                    yield
                    S.cp('act', ww.rearrange("p a b -> p (a b)"), PB[:, 0:256], R=[PBb], W=[wb])
                    S.mm(PB[:, 256:384], ad[:, 1, :], ww[:, 0, :], R=[wb], W=[PBb])
                    if mi < 3:
                        S.mm(PB[:, 384:512], ad[:, 0, :], ww[:, 1, :], R=[wb], W=[PBb])
                    yield
                    if mi < 3:
                        adf = ad.rearrange("p a b -> p (a b)")
                        S.tt('dve', adf, PB[:, 256:512], adf, ALU.add, R=[PBb, wb], W=[wb])
                    else:
                        S.tt('dve', TTr[d_][slot], PB[:, 256:384], ad[:, 0, :], ALU.add, R=[PBb, wb], W=[rbuf])
                    yield

            def scan(d_, si):
                t = order[d_][si]
                ci = d_ * 4 + hd
                ne_ = (negegcF, negegcB)[d_]
                slot = si % RING
                rbuf = rb_[d_][slot]
                tc = slice(t * 128, (t + 1) * 128)
                pA, pAb = ps_t[2 * d_], psb[2 * d_]
                pO, pOb = ps_t[2 * d_ + 1], psb[2 * d_ + 1]
                S.mm(pA[:, 0:128], kTg[:, tc], Sb[d_], R=[Sbuf[d_]], W=[pAb])
                S.mm(pO[:, 0:128], QDr[d_][slot], Sb[d_], start=True, stop=False, R=[Sbuf[d_], rbuf], W=[pOb])
                yield
                S.stt(Xt[d_], pA[:, 0:128], ne_[:, t, ci:ci + 1], vtok[:, t, :], ALU.mult, ALU.add,
                      R=[pAb], W=[xb_[d_]])
                yield
                S.mm(pA[:, 128:256], TTr[d_][slot], Xt[d_], R=[xb_[d_], rbuf], W=[pAb])
                yield
                S.act(vnt[d_], pA[:, 128:256], AF.Copy, scale=btok[:, t, ci:ci + 1], R=[pAb], W=[xb_[d_]])
                yield
                S.mm(pO[:, 0:128], ITr[d_][slot], vnt[d_], start=False, stop=True, R=[xb_[d_], rbuf], W=[pOb])
                S.mm(pA[:, 256:384], KDr[d_][slot], vnt[d_], R=[xb_[d_], rbuf], W=[pAb])
                yield
                if not visited[t]:
                    visited[t] = True
                    S.cp('act', oacc[:, t, :], pO[:, 0:128], R=[pOb], W=[ob[t]])
                else:
                    S.tt('dve', oacc[:, t, :], pO[:, 0:128], oacc[:, t, :], ALU.add, R=[pOb, ob[t]], W=[ob[t]])
                S.stt(Sf[d_], Sf[d_], glast[:, t, ci:ci + 1], pA[:, 256:384], ALU.mult, ALU.add,
                      R=[pAb, Sbuf[d_]], W=[Sbuf[d_]])
                nxt = si + 1
                if nxt < NT:
                    tn_ = order[d_][nxt]
                    if (tn_ // NTS) != (t // NTS):
                        S.ts('dve', Sf[d_], Sf[d_], cf[:, 0:1], None, ALU.mult, R=[Sbuf[d_]], W=[Sbuf[d_]])
                S.cp('pool', Sb[d_], Sf[d_], R=[Sbuf[d_]], W=[Sbuf[d_]])
                yield

            pre_next = [0, 0]
            pre_act = [[], []]
            pre_fin = [0, 0]
            scan_next = [0, 0]
            scan_act = [None, None]
            scan_fin = [0, 0]
            STAG = 11
            while scan_fin[0] < NT or scan_fin[1] < NT:
                for d_ in range(2):
                    if pre_next[d_] < NT and len(pre_act[d_]) < 2 and pre_next[d_] - RING < scan_fin[d_]:
                        if (not pre_act[d_]) or pre_act[d_][0][1] >= STAG:
                            pre_act[d_].append([pre(d_, pre_next[d_]), 0])
                            pre_next[d_] += 1
                    if scan_act[d_] is None and scan_next[d_] < NT and scan_next[d_] < pre_fin[d_]:
                        scan_act[d_] = scan(d_, scan_next[d_])
                        scan_next[d_] += 1
                for d_ in range(2):
                    for ent in list(pre_act[d_]):
                        try:
                            next(ent[0])
                            ent[1] += 1
                        except StopIteration:
                            pre_act[d_].remove(ent)
                            pre_fin[d_] += 1
                    if scan_act[d_] is not None:
                        try:
                            next(scan_act[d_])
                        except StopIteration:
                            scan_act[d_] = None
                            scan_fin[d_] += 1
            sqs = A.alloc([NT], F32)
            fb = Buf()
            zt = [A.alloc([512], F32) for _ in range(2)]
            ztb = bufs(2)
            onb = [A.alloc([128], BF16) for _ in range(2)]
            onbb = bufs(2)
            og = [A.alloc([512], BF16) for _ in range(2)]
            ogb = bufs(2)
            for t in range(NT):
                S.act(Et[0], oacc[:, t, :], AF.Square, R=[ob[t]], W=[wkb[0]])
                S.op('dve', lambda e, o=sqs[:, t:t + 1], i=Et[0]: e.tensor_reduce(o, i, AX.X, ALU.add), [wkb[0]], [fb])
            S.act(sqs, sqs, AF.Sqrt, bias=epsnm[:, 0:1], scale=1.0 / 128.0, R=[fb], W=[fb])
            S.op('dve', lambda e: e.reciprocal(sqs, sqs), [fb], [fb])
            for g in range(NG):
                sl = g % 2
                S.dma('sp', zt[sl], gdnT[1536 + hd * 128:1536 + (hd + 1) * 128, g * 512:(g + 1) * 512], W=[ztb[sl]])
                S.act(zt[sl], zt[sl], AF.Silu, R=[ztb[sl]], W=[ztb[sl]])
                pt, pb = psum()
                pv = pt[:, :].bitcast(BF16).rearrange("p (a b) -> p a b", a=8)
                for ti in range(4):
                    t = g * 4 + ti
                    s2 = ti % 2
                    S.ts('pool', onb[s2], oacc[:, t, :], sqs[:, t:t + 1], None, ALU.mult, R=[ob[t], fb], W=[onbb[s2]])
                    S.tp(pv[:, ti, :], onb[s2], identb, R=[onbb[s2]], W=[pb])
                S.stt(og[sl], pv[:, 0:4, :].rearrange("p a b -> p (a b)"), gw[:, 0:1], zt[sl], ALU.mult, ALU.mult,
                      R=[pb, ztb[sl], pb0], W=[ogb[sl]])
                S.dma('sp', mixT[hd * 128:(hd + 1) * 128, g * 512:(g + 1) * 512], og[sl], R=[ogb[sl]])
            S.barrier()

        A.top = persist_top
        wo = A.alloc([NKT, D], BF16)
        g1b = A.alloc([2, D], F32)
        l1g = A.alloc([D], F32)
        l1b = A.alloc([D], F32)
        sh2 = A.alloc([2, 8], F32)
        sc2 = A.alloc([2, 8], F32)
        rwf = A.alloc([NKT, NE], F32)
        rbb = A.alloc([NE], F32)
        p3 = Buf()
        S.dma('sp', wo, woutb.rearrange("(kt p) n -> p kt n", p=128), W=[p3])
        for s in range(2):
            bcast_row(g1b[:, s, :], modrows[s:s + 1, 2 * D:3 * D], p3)
        bcast_row(l1g, ln1g[L:L + 1, :], p3)
        bcast_row(l1b, ln1b[L:L + 1, :], p3)
        bcast_row(rbb, rb[L:L + 1, :], p3)
        load_mod_cols(sh2, 3, p3)
        load_mod_cols(sc2, 4, p3)
        S.ts('dve', sc2, sc2, 1.0, None, ALU.add, R=[p3], W=[p3])
        S.dma('sp', rwf, rw[L].rearrange("(kt p) n -> p kt n", p=128), W=[p3])
        S.barrier()
        NP = 2
        mx = [A.alloc([NKT, 128], BF16) for _ in range(NP)]
        mxb = bufs(NP)
        xt3 = [A.alloc([D], F32) for _ in range(NP)]
        xt3b = bufs(NP)
        yt3 = [A.alloc([D], F32) for _ in range(NP)]
        yt3b = bufs(NP)
        xm3 = [A.alloc([D], F32) for _ in range(NP)]
        xm3b = bufs(NP)
        xn3 = [A.alloc([D], F32) for _ in range(NP)]
        xn3b = bufs(NP)
        h2f = [A.alloc([NKT, 128], F32) for _ in range(NP)]
        h2fb = bufs(NP)
        h2b = [A.alloc([NKT, 128], BF16) for _ in range(NP)]
        h2bb = bufs(NP)
        st63 = [A.alloc([12], F32) for _ in range(NP)]
        mv3 = [A.alloc([2], F32) for _ in range(NP)]
        rs3 = [A.alloc([1], F32) for _ in range(NP)]
        stb3 = bufs(NP)
        lg = [A.alloc([NE], F32) for _ in range(NP)]
        t8 = [A.alloc([8], F32) for _ in range(NP)]
        mk3 = [A.alloc([NE], F32) for _ in range(NP)]
        ex3 = [A.alloc([NE], F32) for _ in range(NP)]
        sm3 = [A.alloc([1], F32) for _ in range(NP)]
        nm3 = [A.alloc([1], F32) for _ in range(NP)]
        rtb3 = bufs(NP)
        wrb = Buf()
        for t in range(NT):
            sl = t % NP
            seg = t // NTS
            rows = slice(t * 128, (t + 1) * 128)
            S.dma('sp', mx[sl], mixT[:, rows].rearrange("(kt p) n -> p kt n", p=128), W=[mxb[sl]])
            S.dma('pool', xt3[sl], x_src[rows, :], W=[xt3b[sl]])
            for hf in range(2):
                pt, pb = psum()
                for kt in range(NKT):
                    S.mm(pt[:, :], mx[sl][:, kt, :], wo[:, kt, hf * 512:(hf + 1) * 512], start=(kt == 0),
                         stop=(kt == NKT - 1), R=[mxb[sl]], W=[pb])
                S.tt('dve', yt3[sl][:, hf * 512:(hf + 1) * 512], pt[:, :], g1b[:, seg, hf * 512:(hf + 1) * 512],
                     ALU.mult, R=[pb], W=[yt3b[sl]])
            S.stt(yt3[sl], xt3[sl], ALPHA, yt3[sl], ALU.mult, ALU.add, R=[xt3b[sl], yt3b[sl]], W=[yt3b[sl]])
            ln_stats(yt3[sl], yt3b[sl], st63[sl], mv3[sl], rs3[sl], stb3[sl])
            S.ts('dve', yt3[sl], yt3[sl], mv3[sl][:, 0:1], rs3[sl][:, 0:1], ALU.subtract, ALU.mult,
                 R=[yt3b[sl], stb3[sl]], W=[yt3b[sl]])
            S.tt('pool', yt3[sl], yt3[sl], l1g, ALU.mult, R=[yt3b[sl]], W=[yt3b[sl]])
            S.tt('pool', xm3[sl], yt3[sl], l1b, ALU.add, R=[yt3b[sl]], W=[xm3b[sl]])
            S.dma('sp', xmid[rows, :], xm3[sl], R=[xm3b[sl]])
            ln_stats(xm3[sl], xm3b[sl], st63[sl], mv3[sl], rs3[sl], stb3[sl])
            S.ts('dve', xn3[sl], xm3[sl], mv3[sl][:, 0:1], rs3[sl][:, 0:1], ALU.subtract, ALU.mult,
                 R=[xm3b[sl], stb3[sl]], W=[xn3b[sl]])
            for hf in range(2):
                pt, pb = psum()
                for k4 in range(4):
                    kt = hf * 4 + k4
                    S.tp(pt[:, k4 * 128:(k4 + 1) * 128], xn3[sl][:, kt * 128:(kt + 1) * 128], identf,
                         R=[xn3b[sl]], W=[pb])
                for k4 in range(4):
                    kt = hf * 4 + k4
                    S.act(h2f[sl][:, kt, :], pt[:, k4 * 128:(k4 + 1) * 128], AF.Identity,
                          bias=sh2[:, seg, kt:kt + 1], scale=sc2[:, seg, kt:kt + 1], R=[pb], W=[h2fb[sl]])
            S.cp('pool', h2b[sl], h2f[sl], R=[h2fb[sl]], W=[h2bb[sl]])
            S.dma('sp', h2T[:, rows].rearrange("(kt p) n -> p kt n", p=128), h2b[sl], R=[h2bb[sl]])
            pt, pb = psum()
            for kt in range(NKT):
                S.mm(pt[:, 0:NE], h2f[sl][:, kt, :], rwf[:, kt, :], start=(kt == 0), stop=(kt == NKT - 1),
                     R=[h2fb[sl]], W=[pb])
            rb3 = rtb3[sl]
            S.tt('dve', lg[sl], pt[:, 0:NE], rbb, ALU.add, R=[pb], W=[rb3])
            S.op('dve', lambda e, o=t8[sl], i=lg[sl]: e.max(o, i), [rb3], [rb3])
            S.ts('dve', mk3[sl], lg[sl], t8[sl][:, 3:4], None, ALU.is_ge, R=[rb3], W=[rb3])
            S.ts('dve', nm3[sl], t8[sl][:, 0:1], -1.0, None, ALU.mult, R=[rb3], W=[rb3])
            S.act(ex3[sl], lg[sl], AF.Exp, bias=nm3[sl][:, 0:1], scale=1.0, R=[rb3], W=[rb3])
            S.tt('dve', ex3[sl], ex3[sl], mk3[sl], ALU.mult, R=[rb3], W=[rb3])
            S.op('dve', lambda e, o=sm3[sl], i=ex3[sl]: e.tensor_reduce(o, i, AX.X, ALU.add), [rb3], [rb3])
            S.op('dve', lambda e, o=sm3[sl]: e.reciprocal(o, o), [rb3], [rb3])
            S.ts('dve', ex3[sl], ex3[sl], sm3[sl][:, 0:1], None, ALU.mult, R=[rb3], W=[rb3])
            S.dma('sp', wrtD[rows, :], ex3[sl], R=[rb3])
        S.barrier()

        A.top = persist_top
        TC = 1024 if T >= 1024 else T
        NTC = TC // 128
        NCK = T // TC
        hch = A.alloc([NKT, TC], BF16)
        acc4 = A.alloc([NTC, D], F32)
        actT = A.alloc([8, TC], BF16)
        wg_sb = A.alloc([NKT, 2 * DFF], BF16)
        wd_sb = A.alloc([8, D], BF16)
        bgu_sb = A.alloc([NE, 16], F32)
        bd_sb = A.alloc([D], F32)
        g2b = A.alloc([2, D], F32)
        l2g = A.alloc([D], F32)
        l2b = A.alloc([D], F32)
        wrtT = A.alloc([NTC, 128], F32)
        wrt = A.alloc([NTC, NE], F32)
        wrb4 = Buf()
        p4 = Buf()
        S.dma('sp', bgu_sb, bgu[L].rearrange("p (e c) -> p e c", e=NE), W=[p4])
        S.dma('sp', bd_sb[0:NE], bd[L], W=[p4])
        for s in range(2):
            bcast_row(g2b[:, s, :], modrows[s:s + 1, 5 * D:6 * D], p4)
        bcast_row(l2g, ln2g[L:L + 1, :], p4)
        bcast_row(l2b, ln2b[L:L + 1, :], p4)
        S.barrier()
        NQ = 2
        gt_ = [A.alloc([512], F32) for _ in range(NQ)]
        sg_ = [A.alloc([512], F32) for _ in range(NQ)]
        ut_ = [A.alloc([512], F32) for _ in range(NQ)]
        qb4 = bufs(NQ)
        xm4 = [A.alloc([D], F32) for _ in range(2)]
        xm4b = bufs(2)
        y4 = [A.alloc([D], F32) for _ in range(2)]
        y4b = bufs(2)
        st64 = [A.alloc([12], F32) for _ in range(2)]
        mv4 = [A.alloc([2], F32) for _ in range(2)]
        rs4 = [A.alloc([1], F32) for _ in range(2)]
        stb4 = bufs(2)
        hb = Buf()
        wgb_ = Buf()
        wdb_ = Buf()
        acb = bufs(NTC)
        atb = bufs(TC // 512)
        wtb = Buf()
        qc = 0
        for ck in range(NCK):
            tk0 = ck * NTC
            S.dma('sp', hch, h2T[:, ck * TC:(ck + 1) * TC].rearrange("(kt p) n -> p kt n", p=128), W=[hb])
            S.dma('sp', wrt, wrtD[ck * TC:(ck + 1) * TC, :].rearrange("(a p) n -> p a n", p=128), W=[wrb4])
            for tt_ in range(NTC):
                pt, pb = psum()
                S.tp(pt[0:NE, 0:128], wrt[:, tt_, :], identf, R=[wrb4], W=[pb])
                S.cp('act', wrtT[0:NE, tt_, :], pt[0:NE, 0:128], R=[pb], W=[wtb])
                for hf in range(2):
                    pt2, pb2 = psum()
                    S.mm(pt2[:, :], wrtT[0:NE, tt_, :], bd_sb[0:NE, hf * 512:(hf + 1) * 512], R=[wtb, p4], W=[pb2])
                    S.cp('act', acc4[:, tt_, hf * 512:(hf + 1) * 512], pt2[:, :], R=[pb2], W=[acb[tt_]])
            for e in range(NE):
                S.dma('sp', wg_sb, wgub[e].rearrange("(kt p) n -> p kt n", p=128), W=[wgb_])
                S.dma('pool', wd_sb, wdb[e].rearrange("(kt p) n -> p kt n", p=128), W=[wdb_])
                for gi in range(TC // 512):
                    gc0 = gi * 512
                    for c in range(8):
                        pg, pgb = psum()
                        pu, pub = psum()
                        for kt in range(NKT):
                            S.mm(pg[:, :], wg_sb[:, kt, c * 128:(c + 1) * 128], hch[:, kt, gc0:gc0 + 512],
                                 start=(kt == 0), stop=(kt == NKT - 1), R=[wgb_, hb], W=[pgb])
                        for kt in range(NKT):
                            S.mm(pu[:, :], wg_sb[:, kt, DFF + c * 128:DFF + (c + 1) * 128], hch[:, kt, gc0:gc0 + 512],
                                 start=(kt == 0), stop=(kt == NKT - 1), R=[wgb_, hb], W=[pub])
                        q = qc % NQ
                        qc += 1
                        qb = qb4[q]
                        S.ts('dve', gt_[q], pg[:, :], bgu_sb[:, e, c:c + 1], 7.0, ALU.add, ALU.min, R=[pgb], W=[qb])
                        S.act(sg_[q], gt_[q], AF.Sigmoid, scale=1.702, R=[qb], W=[qb])
                        S.ts('dve', ut_[q], pu[:, :], bgu_sb[:, e, 8 + c:9 + c], 7.0, ALU.add, ALU.min, R=[pub], W=[qb])
                        S.ts('pool', ut_[q], ut_[q], -7.0, 1.0, ALU.max, ALU.add, R=[qb], W=[qb])
                        S.tt('pool', gt_[q], gt_[q], sg_[q], ALU.mult, R=[qb], W=[qb])
                        S.tt('dve', actT[:, c, gc0:gc0 + 512], ut_[q], gt_[q], ALU.mult, R=[qb], W=[atb[gi]])
                for tt_ in range(NTC):
                    gi = (tt_ * 128) // 512
                    for hf in range(2):
                        py, pyb = psum()
                        for c in range(8):
                            S.mm(py[:, :], actT[:, c, tt_ * 128:(tt_ + 1) * 128], wd_sb[:, c, hf * 512:(hf + 1) * 512],
                                 start=(c == 0), stop=(c == 7), R=[atb[gi], wdb_], W=[pyb])
                        av = acc4[:, tt_, hf * 512:(hf + 1) * 512]
                        S.stt(av, py[:, :], wrt[:, tt_, e:e + 1], av, ALU.mult, ALU.add,
                              R=[pyb, acb[tt_], wrb4], W=[acb[tt_]])
            for tt_ in range(NTC):
                t = tk0 + tt_
                seg = t // NTS
                sl = tt_ % 2
                rows = slice(t * 128, (t + 1) * 128)
                S.dma('sp', xm4[sl], xmid[rows, :], W=[xm4b[sl]])
                S.tt('pool', y4[sl], acc4[:, tt_, :], g2b[:, seg, :], ALU.mult, R=[acb[tt_]], W=[y4b[sl]])
                S.stt(y4[sl], xm4[sl], ALPHA, y4[sl], ALU.mult, ALU.add, R=[xm4b[sl], y4b[sl]], W=[y4b[sl]])
                ln_stats(y4[sl], y4b[sl], st64[sl], mv4[sl], rs4[sl], stb4[sl])
                S.ts('dve', y4[sl], y4[sl], mv4[sl][:, 0:1], rs4[sl][:, 0:1], ALU.subtract, ALU.mult,
                     R=[y4b[sl], stb4[sl]], W=[y4b[sl]])
                S.tt('pool', y4[sl], y4[sl], l2g, ALU.mult, R=[y4b[sl]], W=[y4b[sl]])
                S.tt('pool', y4[sl], y4[sl], l2b, ALU.add, R=[y4b[sl]], W=[y4b[sl]])
                S.dma('sp', x_dst[rows, :], y4[sl], R=[y4b[sl]])
        S.barrier()

    S.barrier()
    with nc.Block() as block:
        S.emit(block)
    stack.close()
    return nc


def _consts():
    i = np.arange(128)
    identf = np.eye(128, dtype=np.float32)
    onesf = np.ones((128, 128), np.float32)
    cmF = (i[:, None] <= i[None, :]).astype(np.float32)
    cmB = (i[:, None] >= i[None, :]).astype(np.float32)
    negF = np.where(i[None, :] >= i[:, None], 0.0, NEGBIG).astype(np.float32)
    negB = np.where(i[None, :] <= i[:, None], 0.0, NEGBIG).astype(np.float32)
    stF = (i[None, :] > i[:, None]).astype(np.float32)
    stB = (i[None, :] < i[:, None]).astype(np.float32)
    ik = i[:, None]
    j = i[None, :]
    relA = ik - 64 - j
    relB = ik + 64 - j

    def rt(rel, extra_mask):
        ok = (np.abs(rel) <= 64) & extra_mask
        return np.where(ok, -np.abs(rel).astype(np.float32), NEGBIG).astype(np.float32)
    allk = np.ones((128, 128), bool)
    A_I = rt(relA, allk)
    B_I = rt(relB, allk)
    A_F = rt(relA, (ik >= 64) & allk)
    B_L = rt(relB, (ik < 64) & allk)
    I_ = np.concatenate([A_I, B_I], 1)
    F_ = np.concatenate([A_F, B_I], 1)
    L_ = np.concatenate([A_I, B_L], 1)
    pair = lambda a, b: np.concatenate([a, b], 1)
    FI, II, IL, FL = pair(F_, I_), pair(I_, I_), pair(I_, L_), pair(F_, L_)
    rts = np.concatenate([FI, II, IL, FL, II, II, pair(F_, I_), pair(I_, L_)], 1).astype(np.float32)
    blk = lambda s_: (i // s_)
    bd16 = (blk(16)[:, None] == blk(16)[None, :]).astype(np.float32)
    offs = [((blk(2 * s_)[:, None] == blk(2 * s_)[None, :]) & (blk(s_)[:, None] != blk(s_)[None, :])).astype(np.float32) for s_ in (16, 32, 64)]
    bmk = np.concatenate([bd16] + offs, 1)
    sel65 = np.zeros((65, 64), np.float32)
    sel65[64, :] = 1.0
    sels = np.zeros((2, 2, 128), np.float32)
    sels[0, 0] = 1
    sels[1, 1] = 1
    return dict(k_identf=identf, k_onesf=onesf, k_cm=np.concatenate([cmF, cmB], 1),
                k_negm=np.concatenate([negF, negB], 1), k_strict=np.concatenate([stF, stB], 1),
                k_r=rts, k_bm=bmk, k_sel65=sel65, k_sels=sels.reshape(2, 256))


def _weights_map(w, NL):
    f = lambda a: np.ascontiguousarray(np.asarray(a, dtype=np.float32))
    m = {}
    m["w_ada"] = f(w["w_ada"][:NL])
    m["b_ada"] = f(w["b_ada"][:NL])
    m["w_in"] = f(w["w_in"][:NL])
    cw = np.asarray(w["conv_w"][:NL], np.float32)
    m["convw"] = f(cw.reshape(NL, 5, 12, 128).transpose(0, 3, 2, 1).reshape(NL, 128, 60))
    m["alog"] = f(np.asarray(w["a_log"][:NL]).reshape(NL, 8, 1))
    m["dtb"] = f(np.asarray(w["dt_bias"][:NL]).reshape(NL, 8, 1))
    m["gnw"] = f(np.asarray(w["gdn_norm_w"][:NL]).reshape(NL, 128, 1))
    m["w_out"] = f(w["w_out"][:NL])
    m["ln1g"] = f(w["ln1_g"][:NL])
    m["ln1b"] = f(w["ln1_b"][:NL])
    m["rw"] = f(w["router_w"][:NL])
    m["rb"] = f(w["router_b"][:NL])
    m["wgu"] = f(w["w_gate_up"][:NL])
    bg = np.asarray(w["b_gate_up"][:NL], np.float32)
    m["bgu"] = f(bg.reshape(NL, 32, 16, 128).transpose(0, 3, 1, 2).reshape(NL, 128, 512))
    m["wd"] = f(w["w_down"][:NL])
    m["bd"] = f(w["b_down"][:NL])
    m["ln2g"] = f(w["ln2_g"][:NL])
    m["ln2b"] = f(w["ln2_b"][:NL])
    return m


_NC_CACHE = {}


def run_cores(core_inputs, weights, TSEG, NL, debug=False):
    key = (TSEG, NL, debug)
    if key not in _NC_CACHE:
        _NC_CACHE[key] = build(TSEG, NL, debug)
    nc = _NC_CACHE[key]
    base = _weights_map(weights, NL)
    base.update(_consts())
    in_maps = []
    for (x, c2, cfv) in core_inputs:
        m = dict(base)
        m["x"] = np.ascontiguousarray(x, dtype=np.float32)
        m["c2"] = np.ascontiguousarray(c2, dtype=np.float32)
        m["cf"] = np.full((128, 1), cfv, np.float32)
        in_maps.append(m)
    res = run_bass_kernel_spmd(nc, in_maps, core_ids=list(range(len(in_maps))))
    return res


def kernel(x_prompt, x_sample, c_prompt, c_sample, **w):
    x_prompt = np.asarray(x_prompt, np.float32)
    x_sample = np.asarray(x_sample, np.float32)
    c_prompt = np.asarray(c_prompt, np.float32)
    c_sample = np.asarray(c_sample, np.float32)
    TSEG = 4096
    cores = []
    for b in range(4):
        cores.append((x_prompt[b], np.stack([c_prompt[b], c_prompt[b]]), 1.0))
    for pr in range(2):
        xs = np.concatenate([x_sample[2 * pr], x_sample[2 * pr + 1]], 0)
        cores.append((xs, np.stack([c_sample[2 * pr], c_sample[2 * pr + 1]]), 0.0))
    cores.append(cores[4])
    cores.append(cores[5])
    res = run_cores(cores, w, TSEG, 2)
    ys = [np.asarray(r["y"], np.float32) for r in res.results]
    y_prompt = np.stack(ys[0:4], 0)
    y_sample = np.stack([ys[4][:4096], ys[4][4096:], ys[5][:4096], ys[5][4096:]], 0)
    return (y_prompt, y_sample)
```
